# Optimizing a Trainium2 kernel written in Bass

```python
import jax, jax.numpy as jnp
from jax import lax
import numpy as np

D_MODEL = 1024
BATCH = 16
SEQ = 2048
DEPTH = 1

HEAD_DIM = 64
N_HEADS_NA = 8
N_HEADS_DIL = 8
D_NA = N_HEADS_NA * HEAD_DIM
D_DIL = N_HEADS_DIL * HEAD_DIM
D_MIX = D_NA + D_DIL
GRID_W = 64
NA_ROWS_MAX = 8
NA_COLS = 16
DIL_CONFIGS = ((128, 1), (512, 4), (2048, 16))
Q_BLOCK = 128
PEER_HEADS = 8
PEER_NKEYS = 128
PEER_N = PEER_NKEYS * PEER_NKEYS
PEER_DKEY = 256
PEER_TOPK = 16
PEER_TOKEN_BLOCK = 128
EPS = 1e-6

kernel_name = "hybrid_na_dilated_peer_block"


def rmsnorm(x, g):
    xf = x.astype(jnp.float32)
    y = xf * lax.rsqrt(jnp.mean(xf * xf, axis=-1, keepdims=True) + EPS)
    return (y * g.astype(jnp.float32)).astype(x.dtype)


def alibi_slopes(n):
    return (2.0 ** (-8.0 * (np.arange(n) + 1) / n)).astype(np.float32)


def na_index(seq):
    rows = seq // GRID_W
    wr = min(NA_ROWS_MAX, rows)
    t = np.arange(seq)
    r, c = t // GRID_W, t % GRID_W
    r0 = np.clip(r - wr // 2, 0, rows - wr)
    c0 = np.clip(c - NA_COLS // 2, 0, GRID_W - NA_COLS)
    kr = r0[:, None, None] + np.arange(wr)[None, :, None]
    kc = c0[:, None, None] + np.arange(NA_COLS)[None, None, :]
    shape = (seq, wr, NA_COLS)
    idx = np.broadcast_to(kr * GRID_W + kc, shape).reshape(seq, -1)
    dr = np.broadcast_to(kr - r[:, None, None] + NA_ROWS_MAX - 1, shape).reshape(seq, -1)
    dc = np.broadcast_to(kc - c[:, None, None] + NA_COLS - 1, shape).reshape(seq, -1)
    return idx.astype(np.int32), dr.astype(np.int32), dc.astype(np.int32)


def dilated_index(seq, window, dilation):
    half = window // (2 * dilation)
    off = dilation * np.arange(-half, half + 1)
    pos = np.arange(seq)[:, None] + off[None, :]
    valid = (pos >= 0) & (pos < seq)
    idx = np.clip(pos, 0, seq - 1).astype(np.int32)
    return idx, valid, np.abs(off).astype(np.float32)


def gathered_attention(q, k, v, idx, bias, valid):
    B, H, S, hd = q.shape
    nblk = S // Q_BLOCK
    K = idx.shape[-1]
    scale = hd ** -0.5
    q_blocks = jnp.moveaxis(q.reshape(B, H, nblk, Q_BLOCK, hd), 2, 0)
    idx_blocks = idx.reshape(nblk, Q_BLOCK, K)
    bias_blocks = jnp.moveaxis(bias.reshape(H, nblk, Q_BLOCK, K), 1, 0)
    valid_blocks = valid.reshape(nblk, Q_BLOCK, K)

    def block(args):
        qb, ib, bb, mb = args
        kg = jnp.take(k, ib, axis=2)
        vg = jnp.take(v, ib, axis=2)
        s = jnp.einsum('bhqd,bhqkd->bhqk', qb, kg, preferred_element_type=jnp.float32) * scale + bb
        s = jnp.where(mb, s, -jnp.inf)
        lse = jax.nn.logsumexp(s, axis=-1)
        p = jnp.exp(s - lse[..., None])
        o = jnp.einsum('bhqk,bhqkd->bhqd', p.astype(v.dtype), vg)
        return o, lse

    o, lse = lax.map(block, (q_blocks, idx_blocks, bias_blocks, valid_blocks))
    o = jnp.moveaxis(o, 0, 2).reshape(B, H, S, hd)
    lse = jnp.moveaxis(lse, 0, 2).reshape(B, H, S)
    return o, lse


def peer(h, wq, subkeys, u_tab, v_tab):
    B, S, D = h.shape
    T = B * S
    hf = h.reshape(T, D)
    q = (hf @ wq).reshape(T, PEER_HEADS, 2, PEER_DKEY // 2)
    sc = jnp.einsum('thpd,pnd->thpn', q, subkeys, preferred_element_type=jnp.float32)
    top_v, top_i = lax.top_k(sc, PEER_TOPK)
    cand_v = (top_v[:, :, 0, :, None] + top_v[:, :, 1, None, :]).reshape(T, PEER_HEADS, -1)
    cand_id = (top_i[:, :, 0, :, None] * PEER_NKEYS + top_i[:, :, 1, None, :]).reshape(T, PEER_HEADS, -1)
    best_v, best_j = lax.top_k(cand_v, PEER_TOPK)
    ids = jnp.take_along_axis(cand_id, best_j, axis=-1)
    gates = jax.nn.softmax(best_v, axis=-1)
    E = PEER_HEADS * PEER_TOPK
    nblk = T // PEER_TOKEN_BLOCK
    x_blocks = hf.reshape(nblk, PEER_TOKEN_BLOCK, D)
    id_blocks = ids.reshape(nblk, PEER_TOKEN_BLOCK, E)
    g_blocks = gates.reshape(nblk, PEER_TOKEN_BLOCK, E)

    def block(args):
        xb, ib, gb = args
        ub = jnp.take(u_tab, ib, axis=0)
        a = jnp.einsum('td,ted->te', xb, ub, preferred_element_type=jnp.float32)
        w = gb * jax.nn.gelu(a, approximate=False)
        vb = jnp.take(v_tab, ib, axis=0)
        return jnp.einsum('te,ted->td', w.astype(vb.dtype), vb)

    y = lax.map(block, (x_blocks, id_blocks, g_blocks))
    return y.reshape(B, S, D)


def setup_inputs(seed: int = 0) -> dict:
    key = jax.random.key(seed)
    ks = jax.random.split(key, 16)
    f32 = jnp.float32
    L, D = DEPTH, D_MODEL

    def nrm(k, shape, std):
        return jax.random.normal(k, shape, f32) * std

    return {
        "x": nrm(ks[0], (BATCH, SEQ, D), 1.0),
        "c": nrm(ks[1], (BATCH, D), 1.0),
        "ada_w": nrm(ks[2], (L, D, 6 * D), 0.5 * D ** -0.5),
        "ada_b": nrm(ks[3], (L, 6 * D), 0.01),
        "norm1_g": 1.0 + nrm(ks[4], (L, D), 0.02),
        "w_in": nrm(ks[5], (L, D, 3 * D_MIX), D ** -0.5),
        "na_rpb": nrm(ks[6], (L, N_HEADS_NA, 2 * NA_ROWS_MAX - 1, 2 * NA_COLS - 1), 0.1),
        "out_norm_na_g": 1.0 + nrm(ks[7], (L, D_NA), 0.02),
        "out_norm_dil_g": 1.0 + nrm(ks[8], (L, D_DIL), 0.02),
        "w_out": nrm(ks[9], (L, D_MIX, D), D_MIX ** -0.5),
        "norm2_g": 1.0 + nrm(ks[10], (L, D), 0.02),
        "peer_wq": nrm(ks[11], (L, D, PEER_HEADS * PEER_DKEY), D ** -0.5),
        "peer_subkeys": nrm(ks[12], (L, 2, PEER_NKEYS, PEER_DKEY // 2), (PEER_DKEY // 2) ** -0.5),
        "peer_u": nrm(ks[13], (L, PEER_N, D), D ** -0.5),
        "peer_v": nrm(ks[14], (L, PEER_N, D), 1.0),
        "final_g": 1.0 + nrm(ks[15], (D,), 0.02),
    }


def reference(x, c, ada_w, ada_b, norm1_g, w_in, na_rpb, out_norm_na_g, out_norm_dil_g,
              w_out, norm2_g, peer_wq, peer_subkeys, peer_u, peer_v, final_g):
    B, S, D = x.shape

    na_idx_np, na_dr, na_dc = na_index(S)
    na_idx = jnp.asarray(na_idx_np)
    na_valid = jnp.ones(na_idx_np.shape, dtype=bool)
    slopes = alibi_slopes(N_HEADS_DIL)
    dil_tables = []
    for (window, dilation) in DIL_CONFIGS:
        idx_np, valid_np, dist = dilated_index(S, window, dilation)
        bias = np.broadcast_to((-slopes[:, None] * dist[None, :])[:, None, :],
                               (N_HEADS_DIL, S, idx_np.shape[1]))
        dil_tables.append((jnp.asarray(idx_np), jnp.asarray(np.ascontiguousarray(bias)), jnp.asarray(valid_np)))

    def heads(t, n):
        return t.reshape(B, S, n, HEAD_DIM).transpose(0, 2, 1, 3)

    def merge(t):
        return t.transpose(0, 2, 1, 3).reshape(B, S, -1)

    for l in range(DEPTH):
        mod = jax.nn.silu(c) @ ada_w[l] + ada_b[l]
        sh1, sc1, g1, sh2, sc2, g2 = jnp.split(mod[:, None, :], 6, axis=-1)

        h = rmsnorm(x, norm1_g[l]) * (1.0 + sc1) + sh1
        proj = h @ w_in[l]
        qa, ka, va, qd, kd, vd = jnp.split(
            proj, [D_NA, 2 * D_NA, 3 * D_NA, 3 * D_NA + D_DIL, 3 * D_NA + 2 * D_DIL], axis=-1)

        na_bias = na_rpb[l][:, na_dr, na_dc].astype(jnp.float32)
        o_na, _ = gathered_attention(heads(qa, N_HEADS_NA), heads(ka, N_HEADS_NA),
                                     heads(va, N_HEADS_NA), na_idx, na_bias, na_valid)

        qdh, kdh, vdh = heads(qd, N_HEADS_DIL), heads(kd, N_HEADS_DIL), heads(vd, N_HEADS_DIL)
        outs, lses = [], []
        for (idx_d, bias_d, valid_d) in dil_tables:
            o_i, lse_i = gathered_attention(qdh, kdh, vdh, idx_d, bias_d, valid_d)
            outs.append(o_i.astype(jnp.float32))
            lses.append(lse_i)
        alpha = jax.nn.softmax(jnp.stack(lses, 0), axis=0)
        o_dil = jnp.einsum('nbhs,nbhsd->bhsd', alpha, jnp.stack(outs, 0)).astype(x.dtype)

        y = jnp.concatenate([rmsnorm(merge(o_na), out_norm_na_g[l]),
                             rmsnorm(merge(o_dil), out_norm_dil_g[l])], axis=-1) @ w_out[l]
        x = x + g1 * y

        h2 = rmsnorm(x, norm2_g[l]) * (1.0 + sc2) + sh2
        x = x + g2 * peer(h2, peer_wq[l], peer_subkeys[l], peer_u[l], peer_v[l])

    return rmsnorm(x, final_g)
```

```python
import numpy as np
from contextlib import ExitStack
import ml_dtypes
import concourse.bass as bass
import concourse.mybir as mybir
from concourse.bass_utils import run_bass_kernel_spmd

F32 = mybir.dt.float32
BF16 = mybir.dt.bfloat16
U32 = mybir.dt.uint32
AF = mybir.ActivationFunctionType
ALU = mybir.AluOpType
AX = mybir.AxisListType

D = 1024
SEQ = 2048
NT = SEQ // 128
NB = 2
NCORES = 8
EPS = 1e-6
NEG = -32768.0

ENG = ("pe", "act", "dve", "pool", "sp")
EPOCH = 30000
NDMA_SLOTS = 16


class Sched:
    def __init__(self, nc, stack):
        self.nc = nc
        self.stack = stack
        self.ops = {e: [] for e in ENG}
        self.cnt = {e: 0 for e in ENG}
        self.sem = {}
        self.nsem = 0
        self.eng_sem_ids = set()
        for e in ENG:
            self.sem[e] = self._newsem(e)
            self.eng_sem_ids.add(id(self.sem[e]))
        self.dma_sems = {}
        self.dma_n = {}
        self.last_w = {}
        self.readers = {}
        self.seen = {e: {} for e in ENG}
        self.out_tokens = []
        self.latest = {}
        self.defer = None

    def _newsem(self, name):
        self.nsem += 1
        return self.stack.enter_context(self.nc.semaphore(f"s{self.nsem}_{name}"))

    def _deps(self, eng, reads, writes, extra=()):
        deps = list(extra)
        for k in reads:
            t = self.last_w.get(k)
            if t is not None:
                deps.append(t)
        for k in writes:
            t = self.last_w.get(k)
            if t is not None and (t[2] != eng or t[3]):
                deps.append(t)
            for t in self.readers.get(k, ()):
                if t[2] != eng or t[3]:
                    deps.append(t)
        waits = {}
        seen = self.seen[eng]
        for (s, v, e, isdma) in deps:
            if e == eng and eng == "pe" and not isdma:
                continue
            if seen.get(id(s), 0) >= v:
                continue
            if waits.get(id(s), (None, 0))[1] < v:
                waits[id(s)] = (s, v)
        for (s, v) in waits.values():
            seen[id(s)] = v
        return list(waits.values())

    def _commit(self, tok, reads, writes):
        for k in writes:
            self.last_w[k] = tok
            self.readers[k] = []
        for k in reads:
            self.readers.setdefault(k, []).append(tok)
        self.latest[(tok[2], id(tok[0]))] = tok

    def run_deferred(self, lst, n):
        for _ in range(min(n, len(lst))):
            kind, args, kw, cost = lst.pop(0)
            getattr(self, kind)(*args, **kw)

    def run_deferred_budget(self, lst, budget):
        acc = 0.0
        while lst and acc < budget:
            kind, args, kw, cost = lst.pop(0)
            getattr(self, kind)(*args, **kw)
            acc += cost

    def op(self, eng, emit, reads=(), writes=(), extra=(), cost=None):
        if self.defer is not None:
            if cost is None:
                cost = 0.35 if eng == "dve" else 0.0
            self.defer.append(("op", (eng, emit, reads, writes), {}, cost))
            return None
        waits = self._deps(eng, reads, writes, extra)
        if self.cnt[eng] >= EPOCH:
            self.sem[eng] = self._newsem(eng)
            self.eng_sem_ids.add(id(self.sem[eng]))
            self.cnt[eng] = 0
        self.cnt[eng] += 1
        tok = (self.sem[eng], self.cnt[eng], eng, False)
        self.ops[eng].append((waits, emit, tok[0], -self.cnt[eng]))
        self._commit(tok, reads, writes)
        return tok

    def dma(self, eng, emit, reads=(), writes=(), is_output=False, extra=()):
        if self.defer is not None:
            self.defer.append(("dma", (eng, emit, reads, writes, is_output), {}, 0.0))
            return None
        waits = self._deps(eng, reads, writes, extra)
        if eng not in self.dma_sems:
            self.dma_sems[eng] = [self._newsem(f"dma_{eng}{i}") for i in range(NDMA_SLOTS)]
            self.dma_n[eng] = 0
        n = self.dma_n[eng]
        self.dma_n[eng] += 1
        s = self.dma_sems[eng][n % NDMA_SLOTS]
        v = 16 * (n // NDMA_SLOTS + 1)
        if v > 16 and self.seen[eng].get(id(s), 0) < v - 16:
            waits.append((s, v - 16))
            self.seen[eng][id(s)] = v - 16
        tok = (s, v, eng, True)
        self.ops[eng].append((waits, emit, s, 16))
        self._commit(tok, reads, writes)
        if is_output:
            self.out_tokens.append(tok)
        return tok

    def barrier(self):
        toks = list(self.latest.values())
        for e in ENG:
            waits = {}
            for (s, v, te, isdma) in toks:
                if self.seen[e].get(id(s), 0) >= v:
                    continue
                if waits.get(id(s), (None, 0))[1] < v:
                    waits[id(s)] = (s, v)
            for (s, v) in waits.values():
                self.seen[e][id(s)] = v
            self.ops[e].append((list(waits.values()), None, None, 0))
        self.last_w = {}
        self.readers = {}

    def finish(self):
        waits = {}
        for (s, v, e, _) in self.out_tokens:
            if waits.get(id(s), (None, 0))[1] < v:
                waits[id(s)] = (s, v)
        self.ops["sp"].append((list(waits.values()), None, None, 0))

    def emit(self):
        nc = self.nc
        ops = self.ops
        waited = {}
        for e in ENG:
            for (waits, emit, s_, inc) in ops[e]:
                for (ws, wv) in waits:
                    if id(ws) in self.eng_sem_ids:
                        waited.setdefault(id(ws), set()).add(wv)
        rank = {sid: {v: r + 1 for r, v in enumerate(sorted(vals))} for sid, vals in waited.items()}
        with nc.Block() as block:
            def replay(engine, lst):
                for (waits, emit, s_, inc) in lst:
                    for (ws, wv) in waits:
                        if id(ws) in self.eng_sem_ids:
                            engine.wait_ge(ws, rank[id(ws)][wv])
                        else:
                            engine.wait_ge(ws, wv)
                    if emit is None:
                        continue
                    ins = emit(engine)
                    if inc < 0:
                        if -inc in rank.get(id(s_), ()):
                            ins.then_inc(s_, 1)
                    else:
                        ins.then_inc(s_, inc)

            @block.sync
            def _(e):
                replay(e, ops["sp"])

            @block.scalar
            def _(e):
                replay(e, ops["act"])

            @block.vector
            def _(e):
                replay(e, ops["dve"])

            @block.gpsimd
            def _(e):
                replay(e, ops["pool"])

            @block.tensor
            def _(e):
                replay(e, ops["pe"])


def _na_tables():
    rl = np.arange(128) // 64
    cc = np.arange(128) % 64
    masks, mask_ids, plan = [], {}, {}
    for b in range(16):
        r = 2 * b + rl
        r0 = np.clip(r - 4, 0, 24)
        c0 = np.clip(cc - 8, 0, 48)
        lst = []
        for j in range(16):
            kr = 2 * j + rl
            ok = ((kr[:, None] >= r0[None, :]) & (kr[:, None] <= r0[None, :] + 7)
                  & (cc[:, None] >= c0[None, :]) & (cc[:, None] <= c0[None, :] + 15))
            if not ok.any():
                continue
            key = ok.tobytes()
            if key not in mask_ids:
                mask_ids[key] = len(masks)
                masks.append(np.where(ok, 0.0, NEG).astype(np.float32))
            assert -3 <= j - b <= 3
            lst.append((j, j - b, mask_ids[key]))
        plan[b] = lst
    return np.stack(masks), plan


def _na_rpb_index():
    rl = np.arange(128) // 64
    cc = np.arange(128) % 64
    dr = np.zeros((7, 128, 128), np.int64)
    dc = np.zeros((7, 128, 128), np.int64)
    for di, delta in enumerate(range(-3, 4)):
        dr[di] = np.clip(2 * delta + rl[:, None] - rl[None, :] + 7, 0, 14)
        dc[di] = np.clip(cc[:, None] - cc[None, :] + 15, 0, 30)
    return dr, dc


def _dil_tables():
    sl = np.arange(128)[:, None]
    tl = np.arange(128)[None, :]
    logm = np.zeros((17, 2, 128, 128), np.float32)
    for di, delta in enumerate(range(-8, 9)):
        o = 128 * delta + sl - tl
        a = np.abs(o)
        m = (a <= 64).astype(np.float64) + ((a <= 256) & (o % 4 == 0)) + ((a <= 1024) & (o % 16 == 0))
        lm = np.where(m > 0, np.log(np.maximum(m, 1)), NEG).astype(np.float32)
        hi = lm.astype(ml_dtypes.bfloat16).astype(np.float32)
        lo = (lm - hi).astype(ml_dtypes.bfloat16).astype(np.float32)
        logm[di, 0] = hi
        logm[di, 1] = lo
    absl = np.stack([(sl - tl), (tl - sl), np.abs(sl - tl)]).astype(np.float32)
    absc = np.stack([np.full((128, 128), 128.0 * k, np.float32) for k in range(9)])
    slopes = (2.0 ** (-8.0 * (np.arange(8) + 1) / 8)).astype(np.float32)
    nsI = np.stack([-slopes[h] * np.eye(128, dtype=np.float32) for h in range(8)])
    return logm, absl, absc, nsI


NA_MASKS, NA_PLAN = _na_tables()
NM = NA_MASKS.shape[0]


def _bf(a):
    return np.ascontiguousarray(a.astype(ml_dtypes.bfloat16))


def build_nc(debug=False):
    nc = bass.Bass("TRN2", target_bir_lowering=False)

    def din(name, shape, dt=F32):
        return nc.dram_tensor(name, list(shape), dt, kind="ExternalInput").ap()

    x_d = din("x", [NB, SEQ, D])
    c_d = din("c", [NB, D])
    adaw_d = din("ada_w", [D, 6 * D])
    adab_d = din("ada_b", [6 * D])
    n1g_d = din("norm1_g", [D])
    n2g_d = din("norm2_g", [D])
    fg_d = din("final_g", [D])
    og_d = din("og", [D])
    win_d = din("w_in", [D, 3 * D])
    wout_d = din("w_out", [D, D])
    wq_d = din("peer_wq", [D, 2 * D])
    sk_d = din("peer_subkeys", [2, 128, 128])
    u_d = din("peer_u", [16384, D])
    v_d = din("peer_v", [16384, D])
    rpbT_d = din("rpbT", [128, 8, 7, 128], BF16)
    nam_d = din("na_masks", [128, NM, 128], BF16)
    logm_d = din("logm", [128, 17, 2, 128], BF16)
    absl_d = din("absl", [128, 3, 128], BF16)
    absc_d = din("absc", [128, 9, 128], BF16)
    nsI_d = din("nsI", [128, 8, 128], BF16)
    idb_d = din("identb", [128, 128], BF16)
    idf_d = din("identf", [128, 128])
    iota_d = din("iota16", [128, 16])
    out_d = nc.dram_tensor("out", [NB, SEQ, D], F32, kind="ExternalOutput").ap()
    x1_d = nc.dram_tensor("x1s", [NB, SEQ, D], F32,
                          kind="ExternalOutput" if debug else "Internal").ap()
    mods_d = nc.dram_tensor("modscr", [NB, 6, D], F32, kind="Internal").ap()
    uv_d = nc.dram_tensor("uvtab", [16384, 2 * D], BF16, kind="Internal").ap()

    with ExitStack() as st:
        S = Sched(nc, st)

        def sb(name, shape, dt):
            return st.enter_context(nc.sbuf_tensor("sb_" + name, list(shape), dt))

        banks = [st.enter_context(nc.psum_tensor(f"bank{i}", [128, 512], F32)) for i in range(8)]
        Sps = [banks[g][:].rearrange("p (c t) -> p c t", c=4) for g in range(3)]
        Ops2 = [banks[3], banks[4]]
        TPb = banks[5][:].bitcast(BF16).rearrange("p (c t) -> p c t", c=8)
        PJ = [banks[6], banks[7]]

        identb = sb("identb", [128, 128], BF16)
        identf = sb("identf", [128, 128], F32)
        iota16 = sb("iota16", [128, 16], F32)
        thr16 = sb("thr16", [128, 16], F32)
        og_bc = sb("og_bc", [128, D], F32)
        fg_bc = og_bc
        modbc = sb("modbc", [128, 3, D], F32)
        xt = [sb(f"xt{i}", [128, D], F32) for i in range(2)]
        junk = sb("junk", [128, D], BF16)
        hb = sb("hb", [128, D], BF16)
        junkb = hb
        t1 = sb("t1", [128, D], F32)
        stat = sb("stat", [128, 8], F32)
        wbuf = sb("wbuf", [128, 8, 2560], BF16)

        BIGW = 34100
        big = sb("big", [128, BIGW], F32)
        off = [0]

        def carve(words, dt=F32, shape=None):
            a = off[0]
            off[0] += words
            assert off[0] <= BIGW, off[0]
            v = big[:, a:a + words]
            if dt != F32:
                v = v.bitcast(dt)
            return v

        def reset_carve():
            off[0] = 0

        S.dma("sp", lambda e: e.dma_start(out=identb[:], in_=idb_d), writes=["identb"])
        S.dma("sp", lambda e: e.dma_start(out=identf[:], in_=idf_d), writes=["identf"])
        S.dma("sp", lambda e: e.dma_start(out=iota16[:], in_=iota_d), writes=["iota16"])
        S.op("dve", lambda e: e.tensor_scalar(out=thr16[:], in0=iota16[:], scalar1=16.0, scalar2=16.0, op0=ALU.mult, op1=ALU.add),
             reads=["iota16"], writes=["thr16"])
        S.dma("sp", lambda e: e.dma_start(out=og_bc[:], in_=og_d.partition_broadcast(128)), writes=["og_bc"])

        reset_carve()
        c2 = carve(D)
        sc2 = carve(D)
        scT = carve(16)
        scT3 = scT.rearrange("p (k b) -> p k b", k=8)
        adab2 = carve(6 * D)
        modsb = carve(6 * D)
        n1g2 = carve(D)
        n2g2 = carve(D)
        awt = [carve(8 * 512).rearrange("p (k n) -> p k n", k=8) for _ in range(2)]

        S.dma("sp", lambda e: e.dma_start(out=c2[0:2, :], in_=c_d), writes=["c2"])
        S.dma("sp", lambda e: e.dma_start(out=adab2[0:2, :], in_=adab_d.partition_broadcast(2)), writes=["adab2"])
        S.dma("sp", lambda e: e.dma_start(out=n1g2[0:2, :], in_=n1g_d.partition_broadcast(2)), writes=["n1g2"])
        S.dma("sp", lambda e: e.dma_start(out=n2g2[0:2, :], in_=n2g_d.partition_broadcast(2)), writes=["n2g2"])
        S.op("act", lambda e: e.activation(out=sc2[0:2, :], in_=c2[0:2, :], func=AF.Silu), reads=["c2"], writes=["sc2"])
        tpf = banks[5][:, 0:16].rearrange("p (k b) -> p k b", k=8)
        for k in range(8):
            S.op("pe", lambda e, k=k: e.transpose(out=tpf[:, k, :], in_=sc2[0:2, k * 128:(k + 1) * 128],
                                                  identity=identf[0:2, 0:2]),
                 reads=["sc2", "identf"], writes=["TP"])
        S.op("dve", lambda e: e.tensor_copy(out=scT3, in_=tpf), reads=["TP"], writes=["scT"])
        adaw_v = adaw_d.rearrange("(k p) n -> p k n", p=128)
        for n in range(12):
            a = awt[n % 2]
            S.dma("sp", lambda e, a=a, n=n: e.dma_start(out=a, in_=adaw_v[:, :, n * 512:(n + 1) * 512]),
                  writes=[("awt", n % 2)])
            pj = PJ[n % 2]
            for k in range(8):
                S.op("pe", lambda e, a=a, k=k, pj=pj: e.matmul(out=pj[0:2, :], lhsT=scT3[:, k, :], rhs=a[:, k, :],
                                                              start=(k == 0), stop=(k == 7)),
                     reads=["scT", ("awt", n % 2)], writes=[("PJ", n % 2)])
            S.op("dve", lambda e, pj=pj, n=n: e.tensor_tensor(out=modsb[0:2, n * 512:(n + 1) * 512], in0=pj[0:2, :],
                                                              in1=adab2[0:2, n * 512:(n + 1) * 512], op=ALU.add),
                 reads=[("PJ", n % 2), "adab2"], writes=["modsb"])
        S.op("dve", lambda e: e.scalar_tensor_tensor(out=modsb[0:2, D:2 * D], in0=modsb[0:2, D:2 * D], scalar=1.0,
                                                     in1=n1g2[0:2, :], op0=ALU.add, op1=ALU.mult),
             reads=["modsb", "n1g2"], writes=["modsb"])
        S.op("dve", lambda e: e.scalar_tensor_tensor(out=modsb[0:2, 4 * D:5 * D], in0=modsb[0:2, 4 * D:5 * D], scalar=1.0,
                                                     in1=n2g2[0:2, :], op0=ALU.add, op1=ALU.mult),
             reads=["modsb", "n2g2"], writes=["modsb"])
        for dst, src in enumerate([1, 0, 2, 4, 3, 5]):
            S.dma("sp", lambda e, dst=dst, src=src: e.dma_start(out=mods_d[:, dst, :], in_=modsb[0:2, src * D:(src + 1) * D]),
                  reads=["modsb"], writes=["modscr"])
        S.barrier()

        def norm_mod(xin, xkey, gm, sh, mkey, out_bf, out_key, out_f32=None, out_f32_key=None):
            S.op("act", lambda e: e.activation(out=junk[:], in_=xin, func=AF.Square, accum_out=stat[:, 0:1]),
                 reads=[xkey], writes=["junk", "stat"])
            S.op("dve", lambda e: e.tensor_scalar(out=stat[:, 1:2], in0=stat[:, 0:1], scalar1=1.0 / D, scalar2=EPS,
                                                  op0=ALU.mult, op1=ALU.add), reads=["stat"], writes=["stat"])
            S.op("act", lambda e: e.sqrt(out=stat[:, 3:4], in_=stat[:, 1:2]), reads=["stat"], writes=["stat"])
            S.op("dve", lambda e: e.reciprocal(out=stat[:, 2:3], in_=stat[:, 3:4]), reads=["stat"], writes=["stat"])
            S.op("dve", lambda e: e.scalar_tensor_tensor(out=t1[:], in0=xin, scalar=stat[:, 2:3], in1=gm,
                                                         op0=ALU.mult, op1=ALU.mult),
                 reads=[xkey, "stat", mkey], writes=["t1"])
            if out_f32 is not None:
                S.op("dve", lambda e: e.tensor_tensor(out=out_f32, in0=t1[:], in1=sh, op=ALU.add),
                     reads=["t1", ("modbc", 1)], writes=[out_f32_key])
                S.op("dve", lambda e: e.tensor_copy(out=out_bf, in_=out_f32), reads=[out_f32_key], writes=[out_key])
            else:
                S.op("dve", lambda e: e.tensor_tensor(out=out_bf, in0=t1[:], in1=sh, op=ALU.add),
                     reads=["t1", ("modbc", 1)], writes=[out_key])

        reset_carve()
        hT = carve(8 * SEQ // 2, BF16).rearrange("p (k t) -> p k t", k=8)
        KT = carve(4 * SEQ // 2, BF16).rearrange("p (k t) -> p k t", k=4)
        Vt = carve(NT * 8 * 66 // 2, BF16).rearrange("p (i h d) -> p i h d", i=NT, h=8)
        QT = carve(2 * 4 * 512 // 2, BF16).rearrange("p (s k t) -> p s k t", s=2, k=4)
        onTa = carve(4 * SEQ // 2, BF16).rearrange("p (k t) -> p k t", k=4)
        onTd = carve(4 * 128 // 2, BF16).rearrange("p (k t) -> p k t", k=4)
        ETs = [carve(4 * 128 // 2, BF16).rearrange("p (c t) -> p c t", c=4) for _ in range(4)]
        otile = carve(512)
        onb = carve(512 // 2, BF16)
        orec = carve(8)
        rpbT = carve(8 * 7 * 128 // 2, BF16).rearrange("p (h d q) -> p h d q", h=8, d=7)
        nam = carve(NM * 128 // 2, BF16).rearrange("p (m q) -> p m q", m=NM)
        logm = carve(17 * 2 * 128 // 2, BF16).rearrange("p (d s q) -> p d s q", d=17, s=2)
        absl = carve(3 * 128 // 2, BF16).rearrange("p (d q) -> p d q", d=3)
        absc = carve(9 * 128 // 2, BF16).rearrange("p (d q) -> p d q", d=9)
        nsI = carve(8 * 128 // 2, BF16).rearrange("p (h q) -> p h q", h=8)

        print("attn carve words", off[0], flush=True)
        cstg = [carve(2 * D // 2, BF16) for _ in range(2)]
        cblk = [0]

        def conv_step(n):
            for _ in range(n):
                blk = cblk[0]
                if blk >= 128:
                    return
                cblk[0] += 1
                r2 = blk % 2
                rows = slice(blk * 128, (blk + 1) * 128)
                S.dma("pool", lambda e, r2=r2, rows=rows: e.dma_start(out=cstg[r2][:, 0:D], in_=u_d[rows, :]), writes=[("cstg", r2, 0)])
                S.dma("pool", lambda e, r2=r2, rows=rows: e.dma_start(out=cstg[r2][:, D:2 * D], in_=v_d[rows, :]), writes=[("cstg", r2, 1)])
                S.dma("pool", lambda e, r2=r2, rows=rows: e.dma_start(out=uv_d[rows, :], in_=cstg[r2]),
                      reads=[("cstg", r2, 0), ("cstg", r2, 1)], writes=["uvtab"])

        S.dma("sp", lambda e: e.dma_start(out=rpbT, in_=rpbT_d), writes=["rpbT"])
        S.dma("sp", lambda e: e.dma_start(out=nam, in_=nam_d), writes=["nam"])
        S.dma("sp", lambda e: e.dma_start(out=logm, in_=logm_d), writes=["logm"])
        S.dma("sp", lambda e: e.dma_start(out=absl, in_=absl_d), writes=["absl"])
        S.dma("sp", lambda e: e.dma_start(out=absc, in_=absc_d), writes=["absc"])
        S.dma("sp", lambda e: e.dma_start(out=nsI, in_=nsI_d), writes=["nsI"])
        S.op("dve", lambda e: e.memset(Vt[:, :, :, 64:66], 1.0), writes=["Vones"])
        S.op("dve", lambda e: e.memset(QT, 0.0), writes=["QTzero"])

        win_v = win_d.rearrange("(k p) n -> p k n", p=128)
        wout_v = wout_d.rearrange("(k p) n -> p k n", p=128)
        wq_v = wq_d.rearrange("(k p) n -> p k n", p=128)

        def load_w(src_v, c0, ncols, dst0, key):
            for k in range(8):
                for cc in range(0, ncols, 512):
                    S.dma("pool", lambda e, k=k, cc=cc: e.dma_start(out=wbuf[:, k, dst0 + cc:dst0 + cc + 512],
                                                                    in_=src_v[:, k, c0 + cc:c0 + cc + 512]),
                          writes=[("W", k, (dst0 + cc) // 512)])

        evac_flip = [0]

        def evac(out_ap, in_ap, reads, writes, scale=None):
            evac_flip[0] ^= 1
            if evac_flip[0]:
                if scale is None:
                    S.op("act", lambda e: e.copy(out=out_ap, in_=in_ap), reads=reads, writes=writes)
                else:
                    S.op("act", lambda e: e.mul(out_ap, in_ap, scale), reads=reads, writes=writes)
            else:
                if scale is None:
                    S.op("dve", lambda e: e.tensor_copy(out=out_ap, in_=in_ap), reads=reads, writes=writes)
                else:
                    S.op("dve", lambda e: e.tensor_scalar(out=out_ap, in0=in_ap, scalar1=scale, scalar2=None,
                                                          op0=ALU.mult), reads=reads, writes=writes)

        pj_i = [0]

        def proj_fm(dst, dkey, wcol0, tok0, ntok, wkey, scale=None):
            p = pj_i[0] % 2
            pj_i[0] += 1
            pj = PJ[p]
            tiles = sorted(set(range(tok0 // 128, (tok0 + ntok) // 128)))
            for k in range(8):
                S.op("pe", lambda e, k=k, pj=pj: e.matmul(out=pj[:, 0:ntok], lhsT=wbuf[:, k, wcol0:wcol0 + 128],
                                                          rhs=hT[:, k, tok0:tok0 + ntok], start=(k == 0), stop=(k == 7)),
                     reads=[("W", k, wcol0 // 512)] + [("hT", i) for i in tiles], writes=[("PJ", p)])
            if isinstance(dst, list):
                for (d_ap, r0, dk) in dst:
                    evac(d_ap, pj[r0:r0 + 64, 0:ntok], [("PJ", p), "QTzero"], [dk], scale)
            else:
                evac(dst, pj[:, 0:ntok], [("PJ", p)], [dkey], scale)

        def proj_v(b_tile, wcol0, wkey):
            p = pj_i[0] % 2
            pj_i[0] += 1
            pj = PJ[p]
            for k in range(8):
                S.op("pe", lambda e, k=k, pj=pj: e.matmul(out=pj[:, :], lhsT=hT[:, k, b_tile * 128:(b_tile + 1) * 128],
                                                          rhs=wbuf[:, k, wcol0:wcol0 + 512], start=(k == 0), stop=(k == 7)),
                     reads=[("W", k, wcol0 // 512), ("hT", b_tile)], writes=[("PJ", p)])
            evac(Vt[:, b_tile, :, 0:64], pj[:, :].rearrange("p (h d) -> p h d", h=8), [("PJ", p)], [("V", b_tile)])

        sring = [0]

        def attention_tile(i, chunks, bias_for, okey, mid=None):
            iq = (i % 4) * 128
            groups = [chunks[a:a + 4] for a in range(0, len(chunks), 4)]
            units = [(h, gi) for h in range(8) for gi in range(len(groups))]
            ring = {}
            LAG = 2

            def scores(h, gi):
                fc = h // 2
                grp = groups[gi]
                bias_mms = bias_for(h)
                g = sring[0] % 3
                sring[0] += 1
                ring[(h, gi)] = g
                for ci, j in enumerate(grp):
                    S.op("pe", lambda e, g=g, ci=ci, j=j, fc=fc, h=h: e.matmul(out=Sps[g][:, ci, :],
                                                                             lhsT=KT[:, fc, j * 128:(j + 1) * 128],
                                                                             rhs=QT[:, h % 2, fc, iq:iq + 128], start=True, stop=False),
                         reads=[("KT", fc, j // 4), ("QT", fc, h % 2), "QTzero"], writes=[("S", g)])
                    bm = bias_mms(j)
                    for bi, (lt, rh, rk) in enumerate(bm):
                        S.op("pe", lambda e, g=g, ci=ci, lt=lt, rh=rh, last=(bi == len(bm) - 1):
                             e.matmul(out=Sps[g][:, ci, :], lhsT=lt, rhs=rh, start=False, stop=last),
                             reads=rk, writes=[("S", g)])
                n = len(grp)
                S.op("act", lambda e, g=g, n=n: e.activation(out=ETs[g][:, 0:n, :], in_=Sps[g][:, 0:n, :], func=AF.Exp),
                     reads=[("S", g)], writes=[("ET", g)])

            def pv(h, gi):
                grp = groups[gi]
                g = ring[(h, gi)]
                slot = h % 2
                for ci, j in enumerate(grp):
                    first = (gi == 0 and ci == 0)
                    last = (gi == len(groups) - 1 and ci == len(grp) - 1)
                    S.op("pe", lambda e, g=g, ci=ci, j=j, h=h, slot=slot, first=first, last=last: e.matmul(
                        out=Ops2[slot][:, 0:66], lhsT=ETs[g][:, ci, :], rhs=Vt[:, j, h, 0:66], start=first, stop=last),
                        reads=[("ET", g), ("V", j), "Vones"], writes=[("O", slot)])
                if gi == len(groups) - 1:
                    S.op("dve", lambda e, h=h, slot=slot: e.reciprocal(out=orec[:, h:h + 1], in_=Ops2[slot][:, 64:65]),
                         reads=[("O", slot)], writes=["orec"])
                    S.op("dve", lambda e, h=h, slot=slot: e.tensor_scalar(out=otile[:, h * 64:(h + 1) * 64], in0=Ops2[slot][:, 0:64],
                                                                          scalar1=orec[:, h:h + 1], scalar2=None, op0=ALU.mult),
                         reads=[("O", slot), "orec"], writes=[okey])

            for k in range(len(units) + LAG):
                if k < len(units):
                    scores(*units[k])
                if k == LAG and mid is not None:
                    mid()
                if k >= LAG:
                    pv(*units[k - LAG])

        def out_norm(okey, gcol0, dstT, dst_cols, dkey):
            S.op("act", lambda e: e.activation(out=junk[:, 0:512], in_=otile, func=AF.Square, accum_out=stat[:, 4:5]),
                 reads=[okey], writes=["junk", "stat2"])
            S.op("dve", lambda e: e.tensor_scalar(out=stat[:, 5:6], in0=stat[:, 4:5], scalar1=1.0 / 512, scalar2=EPS,
                                                  op0=ALU.mult, op1=ALU.add), reads=["stat2"], writes=["stat2"])
            S.op("act", lambda e: e.sqrt(out=stat[:, 7:8], in_=stat[:, 5:6]), reads=["stat2"], writes=["stat2"])
            S.op("dve", lambda e: e.reciprocal(out=stat[:, 6:7], in_=stat[:, 7:8]), reads=["stat2"], writes=["stat2"])
            S.op("dve", lambda e: e.scalar_tensor_tensor(out=onb, in0=otile, scalar=stat[:, 6:7],
                                                         in1=og_bc[:, gcol0:gcol0 + 512], op0=ALU.mult, op1=ALU.mult),
                 reads=[okey, "stat2", "og_bc"], writes=["onb"])
            for k in range(4):
                S.op("pe", lambda e, k=k: e.transpose(out=TPb[:, k, :], in_=onb[:, k * 128:(k + 1) * 128], identity=identb[:]),
                     reads=["onb", "identb"], writes=["TP"])
            evac(dstT[:, :, dst_cols], TPb[:, 0:4, :], ["TP"], [dkey])

        def na_bias(i, h):
            plan = {j: (dl, mid) for (j, dl, mid) in NA_PLAN[i]}

            def f(j):
                dl, mid = plan[j]
                return [(identb[:], rpbT[:, h, dl + 3, :], ["identb", "rpbT"]),
                        (identb[:], nam[:, mid, :], ["identb", "nam"])]
            return f

        def dil_bias(i, h):
            def f(j):
                dl = j - i
                sg = 0 if dl > 0 else (1 if dl < 0 else 2)
                mm = [(nsI[:, h, :], absl[:, sg, :], ["nsI", "absl"])]
                if dl != 0:
                    mm.append((nsI[:, h, :], absc[:, abs(dl), :], ["nsI", "absc"]))
                mm.append((identb[:], logm[:, dl + 8, 0, :], ["identb", "logm"]))
                if abs(dl) <= 2:
                    mm.append((identb[:], logm[:, dl + 8, 1, :], ["identb", "logm"]))
                return mm
            return f

        xcount = [0]
        epi = [None]
        for b in range(NB):
            for vi in range(3):
                S.dma("sp", lambda e, b=b, vi=vi: e.dma_start(out=modbc[:, vi, :], in_=mods_d[b, vi, :].partition_broadcast(128)),
                      reads=["modscr"], writes=[("modbc", vi)])
            for i in range(NT):
                xb = xt[xcount[0] % 2]
                xk = ("xt", xcount[0] % 2)
                xcount[0] += 1
                S.dma("sp", lambda e, b=b, xb=xb, i=i: e.dma_start(out=xb[:], in_=x_d[b, i * 128:(i + 1) * 128, :]), writes=[xk])
                norm_mod(xb[:], xk, modbc[:, 0, :], modbc[:, 1, :], ("modbc", 0), hb[:], "hb")
                for k in range(8):
                    S.op("pe", lambda e, k=k: e.transpose(out=TPb[:, k, :], in_=hb[:, k * 128:(k + 1) * 128], identity=identb[:]),
                         reads=["hb", "identb"], writes=["TP"])
                evac(hT[:, :, i * 128:(i + 1) * 128], TPb[:, :, :], ["TP"], [("hT", i)])
            load_w(win_v, 0, 1536, 0, "wA")
            for fc in range(4):
                for g4 in range(4):
                    proj_fm(KT[:, fc, g4 * 512:(g4 + 1) * 512], ("KT", fc, g4), 512 + fc * 128, g4 * 512, 512, "wA")
            for i in range(NT):
                proj_v(i, 1024, "wA")
            for i in range(NT):
                if i % 4 == 0:
                    for fc in range(4):
                        proj_fm([(QT[0:64, 0, fc, :], 0, ("QT", fc, 0)), (QT[64:128, 1, fc, :], 64, ("QT", fc, 1))], None, fc * 128, i * 128, 512, "wA", scale=0.125)
                conv_step(2)
                attention_tile(i, [j for (j, _, _) in NA_PLAN[i]], lambda h, i=i: na_bias(i, h), "otile", mid=epi[0])
                epi[0] = (lambda i=i: out_norm("otile", 0, onTa, slice(i * 128, (i + 1) * 128), ("onTa", i)))
            epi[0]()
            epi[0] = None
            load_w(win_v, 1536, 1536, 0, "wA")
            load_w(wout_v, 0, 1024, 1536, "wO")
            for fc in range(4):
                for g4 in range(4):
                    proj_fm(KT[:, fc, g4 * 512:(g4 + 1) * 512], ("KT", fc, g4), 512 + fc * 128, g4 * 512, 512, "wA")
            for i in range(NT):
                proj_v(i, 1024, "wA")
            for i in range(NT):
                if i % 4 == 0:
                    for fc in range(4):
                        proj_fm([(QT[0:64, 0, fc, :], 0, ("QT", fc, 0)), (QT[64:128, 1, fc, :], 64, ("QT", fc, 1))], None, fc * 128, i * 128, 512, "wA", scale=0.125)
                conv_step(2)
                chunks = [j for j in range(NT) if abs(j - i) <= 8]
                attention_tile(i, chunks, lambda h, i=i: dil_bias(i, h), "otile", mid=epi[0])

                def _epi(b=b, i=i):
                    out_norm("otile", 512, onTd, slice(0, 128), "onTd")
                    for half in range(2):
                        pj = PJ[half]
                        for k in range(8):
                            lt = onTa[:, k, i * 128:(i + 1) * 128] if k < 4 else onTd[:, k - 4, :]
                            rk = [("onTa", i)] if k < 4 else ["onTd"]
                            S.op("pe", lambda e, k=k, pj=pj, lt=lt, half=half: e.matmul(
                                out=pj[:, :], lhsT=lt, rhs=wbuf[:, k, 1536 + half * 512:1536 + (half + 1) * 512],
                                start=(k == 0), stop=(k == 7)), reads=rk + [("W", k, 3 + half)], writes=[("PJ", half)])
                    xb = xt[xcount[0] % 2]
                    xk = ("xt", xcount[0] % 2)
                    xcount[0] += 1
                    S.dma("sp", lambda e, b=b, xb=xb, i=i: e.dma_start(out=xb[:], in_=x_d[b, i * 128:(i + 1) * 128, :]), writes=[xk])
                    for half in range(2):
                        S.op("dve", lambda e, half=half: e.tensor_tensor(out=t1[:, half * 512:(half + 1) * 512], in0=PJ[half][:, :],
                                                                          in1=modbc[:, 2, half * 512:(half + 1) * 512], op=ALU.mult),
                             reads=[("PJ", half), ("modbc", 2)], writes=["t1"])
                    S.op("dve", lambda e, xb=xb: e.tensor_tensor(out=xb[:], in0=xb[:], in1=t1[:], op=ALU.add),
                         reads=[xk, "t1"], writes=[xk])
                    S.dma("sp", lambda e, b=b, xb=xb, i=i: e.dma_start(out=x1_d[b, i * 128:(i + 1) * 128, :], in_=xb[:]),
                          reads=[xk], writes=[("x1s", b, i)], is_output=debug)

                epi[0] = _epi
            epi[0]()
            epi[0] = None
        conv_step(128)
        S.barrier()

        reset_carve()
        h2b = [carve(D // 2, BF16) for _ in range(2)]
        h2T = carve(8 * 128 // 2, BF16).rearrange("p (k t) -> p k t", k=8)
        qTs = carve(16 * 128).rearrange("p (c t) -> p c t", c=16)
        skT = carve(2 * 128).rearrange("p (s n) -> p s n", s=2)
        sks = carve(2 * 128).rearrange("p (s n) -> p s n", s=2)
        scs = carve(2048).rearrange("p (g n) -> p g n", g=16)
        sc2_ = carve(128)
        tv = carve(256).rearrange("p (g k) -> p g k", g=16)
        ti = carve(256, U32).rearrange("p (g k) -> p g k", g=16)
        tif = carve(256).rearrange("p (h s k) -> p h s k", h=8, s=2)
        cand = carve(2048).rearrange("p (h a b) -> p h a b", h=8, a=16)
        cand2 = carve(256)
        bv = carve(128).rearrange("p (h k) -> p h k", h=8)
        bp = carve(128, U32).rearrange("p (h k) -> p h k", h=8)
        kif = carve(128).rearrange("p (h k) -> p h k", h=8)
        kjf = carve(128).rearrange("p (h k) -> p h k", h=8)
        eq = cand
        If = carve(128).rearrange("p (h k) -> p h k", h=8)
        Jf = carve(128).rearrange("p (h k) -> p h k", h=8)
        idsf = carve(128)
        ids2 = [carve(128, U32) for _ in range(2)]
        gsum = carve(8)
        gates2 = [carve(128).rearrange("p (h k) -> p h k", h=8) for _ in range(2)]
        adot = carve(128)
        gl = carve(128)
        wg = carve(128)
        NSLOT = 16
        uvg = [carve(2 * D // 2, BF16) for _ in range(NSLOT)]
        prod = [carve(D // 2, BF16) for _ in range(4)]
        dg = [carve(64, BF16) for _ in range(8)]
        modbcP = [modbc, carve(3 * D).rearrange("p (v d) -> p v d", v=3)]

        QP = banks[0][:].rearrange("p (c t) -> p c t", c=4)
        SCP = [banks[1 + q][:].rearrange("p (c t) -> p c t", c=4) for q in range(4)]
        YP = [banks[6], banks[7]]

        load_w(wq_v, 0, 2048, 0, "wQ")
        S.dma("sp", lambda e: e.dma_start(out=fg_bc[:], in_=fg_d.partition_broadcast(128)), writes=["fg_bc"])
        S.dma("sp", lambda e: e.dma_start(out=sks, in_=sk_d.rearrange("s n d -> n s d")), writes=["sks"])
        tpf2 = banks[5][:, 0:256].rearrange("p (s n) -> p s n", s=2)
        for s_ in range(2):
            S.op("pe", lambda e, s_=s_: e.transpose(out=tpf2[:, s_, :], in_=sks[:, s_, :], identity=identf[:]),
                 reads=["sks", "identf"], writes=["TP"])
        S.op("dve", lambda e: e.tensor_copy(out=skT, in_=tpf2), reads=["TP"], writes=["skT"])
        for b in range(NB):
            for vi in range(3):
                S.dma("sp", lambda e, b=b, vi=vi: e.dma_start(out=modbcP[b][:, vi, :], in_=mods_d[b, 3 + vi, :].partition_broadcast(128)),
                      writes=[("modbcP", b, vi)])

        def peer_front(b, i, par):
            xb = xt[par]
            xk = ("xt", par)
            ids = ids2[par]
            gates = gates2[par]
            hbk = ("h2b", par)
            mb = modbcP[b]
            S.dma("sp", lambda e: e.dma_start(out=xb[:], in_=x1_d[b, i * 128:(i + 1) * 128, :]), writes=[xk])
            S.op("act", lambda e: e.activation(out=junk[:], in_=xb[:], func=AF.Square, accum_out=stat[:, 0:1]),
                 reads=[xk], writes=["junk", "stat"])
            S.op("dve", lambda e: e.tensor_scalar(out=stat[:, 1:2], in0=stat[:, 0:1], scalar1=1.0 / D, scalar2=EPS,
                                                  op0=ALU.mult, op1=ALU.add), reads=["stat"], writes=["stat"])
            S.op("act", lambda e: e.sqrt(out=stat[:, 3:4], in_=stat[:, 1:2]), reads=["stat"], writes=["stat"])
            S.op("dve", lambda e: e.reciprocal(out=stat[:, 2:3], in_=stat[:, 3:4]), reads=["stat"], writes=["stat"])
            S.op("dve", lambda e: e.scalar_tensor_tensor(out=t1[:], in0=xb[:], scalar=stat[:, 2:3], in1=mb[:, 0, :],
                                                         op0=ALU.mult, op1=ALU.mult),
                 reads=[xk, "stat", ("modbcP", b, 0)], writes=["t1"])
            S.op("dve", lambda e: e.tensor_tensor(out=h2b[par], in0=t1[:], in1=mb[:, 1, :], op=ALU.add),
                 reads=["t1", ("modbcP", b, 1)], writes=[hbk])
            for k in range(8):
                S.op("pe", lambda e, k=k: e.transpose(out=TPb[:, k, :], in_=h2b[par][:, k * 128:(k + 1) * 128], identity=identb[:]),
                     reads=[hbk, "identb"], writes=["TP"])
            evac(h2T, TPb[:, :, :], ["TP"], ["h2T"])
            for c4 in range(4):
                for cc in range(4):
                    c = c4 * 4 + cc
                    for k in range(8):
                        S.op("pe", lambda e, c=c, cc=cc, k=k: e.matmul(out=QP[:, cc, :], lhsT=wbuf[:, k, c * 128:(c + 1) * 128],
                                                                       rhs=h2T[:, k, :], start=(k == 0), stop=(k == 7)),
                             reads=[("W", k, c // 4), "h2T"], writes=["QP"])
                evac(qTs[:, c4 * 4:(c4 + 1) * 4, :], QP, ["QP"], [("qTs", c4)])
            for q4 in range(4):
                for cc in range(4):
                    c = q4 * 4 + cc
                    S.op("pe", lambda e, c=c, cc=cc, q4=q4: e.matmul(out=SCP[q4][:, cc, :], lhsT=qTs[:, c, :], rhs=skT[:, c % 2, :],
                                                                     start=True, stop=True),
                         reads=[("qTs", q4), "skT"], writes=[("SCP", q4)])
                S.op("act", lambda e, q4=q4: e.copy(out=scs[:, q4 * 4:(q4 + 1) * 4, :], in_=SCP[q4]),
                     reads=[("SCP", q4)], writes=[("scs", q4)])
            for g in range(16):
                rk = [("scs", g // 4)]
                S.op("dve", lambda e, g=g: e.max(out=tv[:, g, 0:8], in_=scs[:, g, :]), reads=rk, writes=["tv"])
                S.op("dve", lambda e, g=g: e.max_index(out=ti[:, g, 0:8], in_max=tv[:, g, 0:8], in_values=scs[:, g, :]),
                     reads=rk + ["tv"], writes=["ti"])
                S.op("dve", lambda e, g=g: e.match_replace(out=sc2_, in_to_replace=tv[:, g, 0:8], in_values=scs[:, g, :],
                                                           imm_value=-1e30), reads=rk + ["tv"], writes=["sc2_"])
                S.op("dve", lambda e, g=g: e.max(out=tv[:, g, 8:16], in_=sc2_), reads=["sc2_"], writes=["tv"])
                S.op("dve", lambda e, g=g: e.max_index(out=ti[:, g, 8:16], in_max=tv[:, g, 8:16], in_values=sc2_),
                     reads=["sc2_", "tv"], writes=["ti"])
            S.op("dve", lambda e: e.tensor_copy(out=tif.rearrange("p h s k -> p (h s k)"),
                                                in_=ti.rearrange("p g k -> p (g k)")), reads=["ti"], writes=["tif"])
            tvv = tv.rearrange("p (h s) k -> p h s k", s=2)
            S.op("dve", lambda e: e.tensor_tensor(out=cand, in0=tvv[:, :, 0, :].unsqueeze(3).to_broadcast([128, 8, 16, 16]),
                                                  in1=tvv[:, :, 1, :].unsqueeze(2).to_broadcast([128, 8, 16, 16]), op=ALU.add),
                 reads=["tv"], writes=["cand"], cost=2.3)
            for h in range(8):
                ch = cand[:, h, :, :].rearrange("p a b -> p (a b)")
                S.op("dve", lambda e, h=h, ch=ch: e.max(out=bv[:, h, 0:8], in_=ch), reads=["cand"], writes=["bv"])
                S.op("dve", lambda e, h=h, ch=ch: e.max_index(out=bp[:, h, 0:8], in_max=bv[:, h, 0:8], in_values=ch),
                     reads=["cand", "bv"], writes=["bp"])
                S.op("dve", lambda e, h=h, ch=ch: e.match_replace(out=cand2, in_to_replace=bv[:, h, 0:8], in_values=ch,
                                                                  imm_value=-1e30), reads=["cand", "bv"], writes=["cand2"])
                S.op("dve", lambda e, h=h: e.max(out=bv[:, h, 8:16], in_=cand2), reads=["cand2"], writes=["bv"])
                S.op("dve", lambda e, h=h: e.max_index(out=bp[:, h, 8:16], in_max=bv[:, h, 8:16], in_values=cand2),
                     reads=["cand2", "bv"], writes=["bp"])
            S.op("dve", lambda e: e.tensor_copy(out=If, in_=bp), reads=["bp"], writes=["If"])
            thb = thr16[:].unsqueeze(1).unsqueeze(1).to_broadcast([128, 8, 16, 16])
            S.op("dve", lambda e: e.tensor_tensor(out=eq, in0=If.unsqueeze(3).to_broadcast([128, 8, 16, 16]), in1=thb, op=ALU.is_ge),
                 reads=["If", "thr16"], writes=["eq"], cost=2.3)
            S.op("dve", lambda e: e.tensor_reduce(out=kif, in_=eq, axis=AX.X, op=ALU.add), reads=["eq"], writes=["kif"], cost=2.3)
            S.op("dve", lambda e: e.scalar_tensor_tensor(out=kjf, in0=kif, scalar=-16.0, in1=If, op0=ALU.mult, op1=ALU.add),
                 reads=["kif", "If"], writes=["kjf"])
            iob = iota16[:].unsqueeze(1).unsqueeze(1).to_broadcast([128, 8, 16, 16])
            for (kf, kkey, sidx, dst, dkey) in ((kif, "kif", 0, If, "If"), (kjf, "kjf", 1, Jf, "Jf")):
                S.op("dve", lambda e, kf=kf: e.tensor_tensor(out=eq, in0=kf.unsqueeze(3).to_broadcast([128, 8, 16, 16]),
                                                             in1=iob, op=ALU.is_equal), reads=[kkey, "iota16"], writes=["eq"], cost=2.3)
                S.op("dve", lambda e, sidx=sidx: e.tensor_tensor(out=eq, in0=eq,
                                                                 in1=tif[:, :, sidx, :].unsqueeze(2).to_broadcast([128, 8, 16, 16]),
                                                                 op=ALU.mult), reads=["eq", "tif"], writes=["eq"], cost=2.3)
                S.op("dve", lambda e, dst=dst: e.tensor_reduce(out=dst, in_=eq, axis=AX.X, op=ALU.add),
                     reads=["eq", "kjf"], writes=[dkey], cost=2.3)
            S.op("dve", lambda e: e.scalar_tensor_tensor(out=idsf, in0=If.rearrange("p h k -> p (h k)"), scalar=128.0,
                                                         in1=Jf.rearrange("p h k -> p (h k)"), op0=ALU.mult, op1=ALU.add),
                 reads=["If", "Jf"], writes=["idsf"])
            S.op("dve", lambda e: e.tensor_scalar(out=idsf, in0=idsf, scalar1=16383.0, scalar2=0.0, op0=ALU.min, op1=ALU.max),
                 reads=["idsf"], writes=["idsf"])
            S.op("dve", lambda e: e.tensor_copy(out=ids, in_=idsf), reads=["idsf"], writes=[("ids", par)])
            gk = ("gates", par)
            S.op("dve", lambda e: e.tensor_tensor(out=gates, in0=bv, in1=bv[:, :, 0:1].to_broadcast([128, 8, 16]), op=ALU.subtract),
                 reads=["bv"], writes=[gk])
            S.op("act", lambda e: e.activation(out=gates, in_=gates, func=AF.Exp), reads=[gk], writes=[gk])
            S.op("dve", lambda e: e.tensor_reduce(out=gsum, in_=gates, axis=AX.X, op=ALU.add), reads=[gk], writes=["gsum"])
            S.op("dve", lambda e: e.reciprocal(out=gsum, in_=gsum), reads=["gsum"], writes=["gsum"])
            S.op("dve", lambda e: e.tensor_tensor(out=gates, in0=gates, in1=gsum.unsqueeze(2).to_broadcast([128, 8, 16]), op=ALU.mult),
                 reads=[gk, "gsum"], writes=[gk])

        tiles = [(b, i) for b in range(NB) for i in range(NT)]
        S.defer = []
        peer_front(tiles[0][0], tiles[0][1], 0)
        pending = S.defer
        S.defer = None
        S.run_deferred(pending, len(pending))
        gcount = [0]
        pcount = [0]
        print('peer carve words', off[0], flush=True)
        for ti_, (b, i) in enumerate(tiles):
            par = ti_ % 2
            nxt = []
            if ti_ + 1 < len(tiles):
                S.defer = []
                peer_front(tiles[ti_ + 1][0], tiles[ti_ + 1][1], 1 - par)
                nxt = S.defer
                S.defer = None
            per_slot = (len(nxt) + 111) // 112
            budget = sum(t[3] for t in nxt) / 27.0 if nxt else 0.0
            ids = ids2[par]
            gates = gates2[par].rearrange("p h k -> p (h k)")
            GB = 4

            def gathers(e0):
                sls = []
                for e_ in range(e0, e0 + GB):
                    sl = gcount[0] % NSLOT
                    gcount[0] += 1
                    sls.append(sl)
                    S.dma("pool", lambda e, e_=e_, sl=sl, ids=ids: e.indirect_dma_start(
                        out=uvg[sl], out_offset=None, in_=uv_d, in_offset=bass.IndirectOffsetOnAxis(ap=ids[:, e_:e_ + 1], axis=0)),
                        reads=[("ids", par)], writes=[("uvg", sl)])
                return sls

            def dots(e0, sls):
                for n_, e_ in enumerate(range(e0, e0 + GB)):
                    sl = sls[n_]
                    ak = ("adot", (e_ // GB) % 2, e_ % 4)
                    pr = pcount[0] % 4
                    pcount[0] += 1
                    S.op("dve", lambda e, sl=sl, par=par, pr=pr: e.tensor_tensor(out=prod[pr], in0=uvg[sl][:, 0:D], in1=h2b[par], op=ALU.mult),
                         reads=[("uvg", sl), ("h2b", par)], writes=[("prod", pr)])
                    S.op("act", lambda e, e_=e_, pr=pr: e.activation(out=junk[:], in_=prod[pr], func=AF.Copy, accum_out=adot[:, e_:e_ + 1]),
                         reads=[("prod", pr)], writes=["junk", ak])

            def gelu_stage(e0, sls):
                gb = (e0 // GB) % 2
                S.op("act", lambda e, e0=e0: e.activation(out=gl[:, e0:e0 + GB], in_=adot[:, e0:e0 + GB], func=AF.Gelu),
                     reads=[("adot", gb, 0), ("adot", gb, 1), ("adot", gb, 2), ("adot", gb, 3)], writes=[("gl", gb)])

            def combine(e0, sls):
                gb = (e0 // GB) % 2
                S.op("dve", lambda e, e0=e0, gates=gates: e.tensor_tensor(out=wg[:, e0 + 2:e0 + 4], in0=gl[:, e0 + 2:e0 + 4],
                                                                           in1=gates[:, e0 + 2:e0 + 4], op=ALU.mult),
                     reads=[("gl", gb), ("gates", par)], writes=[("wg", gb)])
                for n_, e_ in enumerate(range(e0, e0 + GB)):
                    sl = sls[n_]
                    d_ = e_ % 8
                    if n_ < 2:
                        S.op("dve", lambda e, e_=e_, d_=d_, gates=gates: e.tensor_scalar(out=dg[d_], in0=identb[:], scalar1=gl[:, e_:e_ + 1],
                                                                                         scalar2=gates[:, e_:e_ + 1], op0=ALU.mult, op1=ALU.mult),
                             reads=[("gl", gb), ("gates", par), "identb"], writes=[("dg", d_)])
                    else:
                        S.op("act", lambda e, e_=e_, d_=d_: e.activation(out=dg[d_], in_=identb[:], func=AF.Copy, scale=wg[:, e_:e_ + 1]),
                             reads=[("wg", gb), "identb"], writes=[("dg", d_)])
                    for half in range(2):
                        S.op("pe", lambda e, e_=e_, sl=sl, d_=d_, half=half: e.matmul(
                            out=YP[half][:, :], lhsT=dg[d_], rhs=uvg[sl][:, D + half * 512:D + (half + 1) * 512],
                            start=(e_ == 0), stop=(e_ == 127)), reads=[("dg", d_), ("uvg", sl)], writes=[("YP", half)])

            prev = None
            for e0 in range(0, 128, GB):
                sls = gathers(e0)
                if prev is not None:
                    gelu_stage(*prev)
                dots(e0, sls)
                if prev is not None:
                    combine(*prev)
                prev = (e0, sls)
                if e0 >= 8:
                    S.run_deferred_budget(nxt, budget)
            gelu_stage(*prev)
            combine(*prev)
            S.run_deferred(nxt, len(nxt))
            xb = xt[par]
            xk = ("xt", par)
            mb = modbcP[b]
            for half in range(2):
                S.op("dve", lambda e, half=half, mb=mb: e.tensor_tensor(out=t1[:, half * 512:(half + 1) * 512], in0=YP[half][:, :],
                                                                         in1=mb[:, 2, half * 512:(half + 1) * 512], op=ALU.mult),
                     reads=[("YP", half), ("modbcP", b, 2)], writes=["t1"])
            S.op("dve", lambda e, xb=xb: e.tensor_tensor(out=xb[:], in0=xb[:], in1=t1[:], op=ALU.add),
                 reads=[xk, "t1"], writes=[xk])
            S.op("act", lambda e, xb=xb: e.activation(out=junk[:], in_=xb[:], func=AF.Square, accum_out=stat[:, 4:5]),
                 reads=[xk], writes=["junk", "stat2"])
            S.op("dve", lambda e: e.tensor_scalar(out=stat[:, 5:6], in0=stat[:, 4:5], scalar1=1.0 / D, scalar2=EPS,
                                                  op0=ALU.mult, op1=ALU.add), reads=["stat2"], writes=["stat2"])
            S.op("act", lambda e: e.sqrt(out=stat[:, 7:8], in_=stat[:, 5:6]), reads=["stat2"], writes=["stat2"])
            S.op("dve", lambda e: e.reciprocal(out=stat[:, 6:7], in_=stat[:, 7:8]), reads=["stat2"], writes=["stat2"])
            S.op("dve", lambda e, xb=xb: e.scalar_tensor_tensor(out=xb[:], in0=xb[:], scalar=stat[:, 6:7], in1=fg_bc[:],
                                                                op0=ALU.mult, op1=ALU.mult),
                 reads=[xk, "stat2", "fg_bc"], writes=[xk])
            S.dma("sp", lambda e, b=b, i=i, xb=xb: e.dma_start(out=out_d[b, i * 128:(i + 1) * 128, :], in_=xb[:]),
                  reads=[xk], writes=[("out", b, i)], is_output=True)
        S.finish()
        S.emit()
        print("ops per engine:", {k: len(v) for k, v in S.ops.items()}, "sems:", S.nsem, flush=True)
    return nc


_CONSTS = None


def _consts(na_rpb):
    global _CONSTS
    if _CONSTS is None:
        logm, absl, absc, nsI = _dil_tables()
        dr, dc = _na_rpb_index()
        _CONSTS = dict(
            na_masks=_bf(NA_MASKS.transpose(1, 0, 2)),
            logm=_bf(logm.transpose(2, 0, 1, 3)),
            absl=_bf(absl.transpose(1, 0, 2)),
            absc=_bf(absc.transpose(1, 0, 2)),
            nsI=_bf(nsI.transpose(1, 0, 2)),
            identb=_bf(np.eye(128, dtype=np.float32)),
            identf=np.eye(128, dtype=np.float32),
            iota16=np.ascontiguousarray(np.broadcast_to(np.arange(16, dtype=np.float32), (128, 16))),
            _dr=dr, _dc=dc,
        )
    cst = {k: v for k, v in _CONSTS.items() if not k.startswith("_")}
    rp = np.asarray(na_rpb, np.float32)[0]
    g = rp[:, _CONSTS["_dr"], _CONSTS["_dc"]]
    cst["rpbT"] = _bf(g.transpose(2, 0, 1, 3))
    return cst


def kernel(x, c, ada_w, ada_b, norm1_g, w_in, na_rpb, out_norm_na_g, out_norm_dil_g, w_out, norm2_g,
           peer_wq, peer_subkeys, peer_u, peer_v, final_g, _debug=False, _cores=None):
    f = lambda a: np.ascontiguousarray(np.asarray(a, dtype=np.float32))
    x = f(x); c = f(c)
    shared = dict(
        ada_w=f(ada_w)[0], ada_b=f(ada_b)[0], norm1_g=f(norm1_g)[0], norm2_g=f(norm2_g)[0], final_g=f(final_g),
        og=np.ascontiguousarray(np.concatenate([f(out_norm_na_g)[0], f(out_norm_dil_g)[0]])),
        w_in=f(w_in)[0], w_out=f(w_out)[0], peer_wq=f(peer_wq)[0], peer_subkeys=f(peer_subkeys)[0],
        peer_u=f(peer_u)[0], peer_v=f(peer_v)[0],
    )
    shared.update(_consts(na_rpb))
    cores = list(range(NCORES)) if _cores is None else _cores
    nc = build_nc(debug=_debug)
    in_maps = []
    for ci in cores:
        m = dict(shared)
        m["x"] = np.ascontiguousarray(x[ci * NB:(ci + 1) * NB])
        m["c"] = np.ascontiguousarray(c[ci * NB:(ci + 1) * NB])
        in_maps.append(m)
    res = run_bass_kernel_spmd(nc, in_maps, core_ids=list(range(len(cores))))
    out = np.concatenate([np.asarray(r["out"]) for r in res.results], axis=0).astype(np.float32)
    if _debug:
        return out, [r for r in res.results]
    return out
```

```python
import numpy as np
from contextlib import ExitStack
import ml_dtypes
import concourse.bass as bass
import concourse.mybir as mybir
from concourse.bass_utils import run_bass_kernel_spmd

F32 = mybir.dt.float32
BF16 = mybir.dt.bfloat16
U32 = mybir.dt.uint32
AF = mybir.ActivationFunctionType
ALU = mybir.AluOpType
AX = mybir.AxisListType

D = 1024
SEQ = 2048
NT = SEQ // 128
NB = 2
NCORES = 8
EPS = 1e-6
NEG = -32768.0

ENG = ("pe", "act", "dve", "pool", "sp")
EPOCH = 30000
NDMA_SLOTS = 16


class Sched:
    def __init__(self, nc, stack):
        self.nc = nc
        self.stack = stack
        self.ops = {e: [] for e in ENG}
        self.cnt = {e: 0 for e in ENG}
        self.sem = {}
        self.nsem = 0
        self.eng_sem_ids = set()
        for e in ENG:
            self.sem[e] = self._newsem(e)
            self.eng_sem_ids.add(id(self.sem[e]))
        self.dma_sems = {}
        self.dma_n = {}
        self.last_w = {}
        self.readers = {}
        self.seen = {e: {} for e in ENG}
        self.out_tokens = []
        self.latest = {}
        self.defer = None

    def _newsem(self, name):
        self.nsem += 1
        return self.stack.enter_context(self.nc.semaphore(f"s{self.nsem}_{name}"))

    def _deps(self, eng, reads, writes, extra=()):
        deps = list(extra)
        for k in reads:
            t = self.last_w.get(k)
            if t is not None:
                deps.append(t)
        for k in writes:
            t = self.last_w.get(k)
            if t is not None and (t[2] != eng or t[3]):
                deps.append(t)
            for t in self.readers.get(k, ()):
                if t[2] != eng or t[3]:
                    deps.append(t)
        waits = {}
        seen = self.seen[eng]
        for (s, v, e, isdma) in deps:
            if e == eng and eng == "pe" and not isdma:
                continue
            if seen.get(id(s), 0) >= v:
                continue
            if waits.get(id(s), (None, 0))[1] < v:
                waits[id(s)] = (s, v)
        for (s, v) in waits.values():
            seen[id(s)] = v
        return list(waits.values())

    def _commit(self, tok, reads, writes):
        for k in writes:
            self.last_w[k] = tok
            self.readers[k] = []
        for k in reads:
            self.readers.setdefault(k, []).append(tok)
        self.latest[(tok[2], id(tok[0]))] = tok

    def run_deferred(self, lst, n):
        for _ in range(min(n, len(lst))):
            kind, args, kw, cost = lst.pop(0)
            getattr(self, kind)(*args, **kw)

    def run_deferred_budget(self, lst, budget):
        acc = 0.0
        while lst and acc < budget:
            kind, args, kw, cost = lst.pop(0)
            getattr(self, kind)(*args, **kw)
            acc += cost

    def op(self, eng, emit, reads=(), writes=(), extra=(), cost=None):
        if self.defer is not None:
            if cost is None:
                cost = 0.35 if eng == "dve" else 0.0
            self.defer.append(("op", (eng, emit, reads, writes), {}, cost))
            return None
        waits = self._deps(eng, reads, writes, extra)
        if self.cnt[eng] >= EPOCH:
            self.sem[eng] = self._newsem(eng)
            self.eng_sem_ids.add(id(self.sem[eng]))
            self.cnt[eng] = 0
        self.cnt[eng] += 1
        tok = (self.sem[eng], self.cnt[eng], eng, False)
        self.ops[eng].append((waits, emit, tok[0], -self.cnt[eng]))
        self._commit(tok, reads, writes)
        return tok

    def dma(self, eng, emit, reads=(), writes=(), is_output=False, extra=()):
        if self.defer is not None:
            self.defer.append(("dma", (eng, emit, reads, writes, is_output), {}, 0.0))
            return None
        waits = self._deps(eng, reads, writes, extra)
        if eng not in self.dma_sems:
            self.dma_sems[eng] = [self._newsem(f"dma_{eng}{i}") for i in range(NDMA_SLOTS)]
            self.dma_n[eng] = 0
        n = self.dma_n[eng]
        self.dma_n[eng] += 1
        s = self.dma_sems[eng][n % NDMA_SLOTS]
        v = 16 * (n // NDMA_SLOTS + 1)
        if v > 16 and self.seen[eng].get(id(s), 0) < v - 16:
            waits.append((s, v - 16))
            self.seen[eng][id(s)] = v - 16
        tok = (s, v, eng, True)
        self.ops[eng].append((waits, emit, s, 16))
        self._commit(tok, reads, writes)
        if is_output:
            self.out_tokens.append(tok)
        return tok

    def barrier(self):
        toks = list(self.latest.values())
        for e in ENG:
            waits = {}
            for (s, v, te, isdma) in toks:
                if self.seen[e].get(id(s), 0) >= v:
                    continue
                if waits.get(id(s), (None, 0))[1] < v:
                    waits[id(s)] = (s, v)
            for (s, v) in waits.values():
                self.seen[e][id(s)] = v
            self.ops[e].append((list(waits.values()), None, None, 0))
        self.last_w = {}
        self.readers = {}

    def finish(self):
        waits = {}
        for (s, v, e, _) in self.out_tokens:
            if waits.get(id(s), (None, 0))[1] < v:
                waits[id(s)] = (s, v)
        self.ops["sp"].append((list(waits.values()), None, None, 0))

    def emit(self):
        nc = self.nc
        ops = self.ops
        waited = {}
        for e in ENG:
            for (waits, emit, s_, inc) in ops[e]:
                for (ws, wv) in waits:
                    if id(ws) in self.eng_sem_ids:
                        waited.setdefault(id(ws), set()).add(wv)
        rank = {sid: {v: r + 1 for r, v in enumerate(sorted(vals))} for sid, vals in waited.items()}
        with nc.Block() as block:
            def replay(engine, lst):
                for (waits, emit, s_, inc) in lst:
                    for (ws, wv) in waits:
                        if id(ws) in self.eng_sem_ids:
                            engine.wait_ge(ws, rank[id(ws)][wv])
                        else:
                            engine.wait_ge(ws, wv)
                    if emit is None:
                        continue
                    ins = emit(engine)
                    if inc < 0:
                        if -inc in rank.get(id(s_), ()):
                            ins.then_inc(s_, 1)
                    else:
                        ins.then_inc(s_, inc)

            @block.sync
            def _(e):
                replay(e, ops["sp"])

            @block.scalar
            def _(e):
                replay(e, ops["act"])

            @block.vector
            def _(e):
                replay(e, ops["dve"])

            @block.gpsimd
            def _(e):
                replay(e, ops["pool"])

            @block.tensor
            def _(e):
                replay(e, ops["pe"])


def _na_tables():
    rl = np.arange(128) // 64
    cc = np.arange(128) % 64
    masks, mask_ids, plan = [], {}, {}
    for b in range(16):
        r = 2 * b + rl
        r0 = np.clip(r - 4, 0, 24)
        c0 = np.clip(cc - 8, 0, 48)
        lst = []
        for j in range(16):
            kr = 2 * j + rl
            ok = ((kr[:, None] >= r0[None, :]) & (kr[:, None] <= r0[None, :] + 7)
                  & (cc[:, None] >= c0[None, :]) & (cc[:, None] <= c0[None, :] + 15))
            if not ok.any():
                continue
            key = ok.tobytes()
            if key not in mask_ids:
                mask_ids[key] = len(masks)
                masks.append(np.where(ok, 0.0, NEG).astype(np.float32))
            assert -3 <= j - b <= 3
            lst.append((j, j - b, mask_ids[key]))
        plan[b] = lst
    return np.stack(masks), plan


def _na_rpb_index():
    rl = np.arange(128) // 64
    cc = np.arange(128) % 64
    dr = np.zeros((7, 128, 128), np.int64)
    dc = np.zeros((7, 128, 128), np.int64)
    for di, delta in enumerate(range(-3, 4)):
        dr[di] = np.clip(2 * delta + rl[:, None] - rl[None, :] + 7, 0, 14)
        dc[di] = np.clip(cc[:, None] - cc[None, :] + 15, 0, 30)
    return dr, dc


def _dil_tables():
    sl = np.arange(128)[:, None]
    tl = np.arange(128)[None, :]
    logm = np.zeros((17, 2, 128, 128), np.float32)
    for di, delta in enumerate(range(-8, 9)):
        o = 128 * delta + sl - tl
        a = np.abs(o)
        m = (a <= 64).astype(np.float64) + ((a <= 256) & (o % 4 == 0)) + ((a <= 1024) & (o % 16 == 0))
        lm = np.where(m > 0, np.log(np.maximum(m, 1)), NEG).astype(np.float32)
        hi = lm.astype(ml_dtypes.bfloat16).astype(np.float32)
        lo = (lm - hi).astype(ml_dtypes.bfloat16).astype(np.float32)
        logm[di, 0] = hi
        logm[di, 1] = lo
    absl = np.stack([(sl - tl), (tl - sl), np.abs(sl - tl)]).astype(np.float32)
    absc = np.stack([np.full((128, 128), 128.0 * k, np.float32) for k in range(9)])
    slopes = (2.0 ** (-8.0 * (np.arange(8) + 1) / 8)).astype(np.float32)
    nsI = np.stack([-slopes[h] * np.eye(128, dtype=np.float32) for h in range(8)])
    return logm, absl, absc, nsI


NA_MASKS, NA_PLAN = _na_tables()
NM = NA_MASKS.shape[0]


def _bf(a):
    return np.ascontiguousarray(a.astype(ml_dtypes.bfloat16))


def build_nc(debug=False):
    nc = bass.Bass("TRN2", target_bir_lowering=False)

    def din(name, shape, dt=F32):
        return nc.dram_tensor(name, list(shape), dt, kind="ExternalInput").ap()

    x_d = din("x", [NB, SEQ, D])
    c_d = din("c", [NB, D])
    adaw_d = din("ada_w", [D, 6 * D])
    adab_d = din("ada_b", [6 * D])
    n1g_d = din("norm1_g", [D])
    n2g_d = din("norm2_g", [D])
    fg_d = din("final_g", [D])
    og_d = din("og", [D])
    win_d = din("w_in", [D, 3 * D])
    wout_d = din("w_out", [D, D])
    wq_d = din("peer_wq", [D, 2 * D])
    sk_d = din("peer_subkeys", [2, 128, 128])
    u_d = din("peer_u", [16384, D])
    v_d = din("peer_v", [16384, D])
    rpbT_d = din("rpbT", [128, 8, 7, 128], BF16)
    nam_d = din("na_masks", [128, NM, 128], BF16)
    logm_d = din("logm", [128, 17, 2, 128], BF16)
    absl_d = din("absl", [128, 3, 128], BF16)
    absc_d = din("absc", [128, 9, 128], BF16)
    nsI_d = din("nsI", [128, 8, 128], BF16)
    idb_d = din("identb", [128, 128], BF16)
    idf_d = din("identf", [128, 128])
    iota_d = din("iota16", [128, 16])
    out_d = nc.dram_tensor("out", [NB, SEQ, D], F32, kind="ExternalOutput").ap()
    x1_d = nc.dram_tensor("x1s", [NB, SEQ, D], F32,
                          kind="ExternalOutput" if debug else "Internal").ap()
    mods_d = nc.dram_tensor("modscr", [NB, 6, D], F32, kind="Internal").ap()
    uv_d = nc.dram_tensor("uvtab", [16384, 2 * D], BF16, kind="Internal").ap()

    with ExitStack() as st:
        S = Sched(nc, st)

        def sb(name, shape, dt):
            return st.enter_context(nc.sbuf_tensor("sb_" + name, list(shape), dt))

        banks = [st.enter_context(nc.psum_tensor(f"bank{i}", [128, 512], F32)) for i in range(8)]
        Sps = [banks[g][:].rearrange("p (c t) -> p c t", c=4) for g in range(3)]
        Ops2 = [banks[3], banks[4]]
        TPb = banks[5][:].bitcast(BF16).rearrange("p (c t) -> p c t", c=8)
        PJ = [banks[6], banks[7]]

        identb = sb("identb", [128, 128], BF16)
        identf = sb("identf", [128, 128], F32)
        iota16 = sb("iota16", [128, 16], F32)
        thr16 = sb("thr16", [128, 16], F32)
        og_bc = sb("og_bc", [128, D], F32)
        fg_bc = og_bc
        modbc = sb("modbc", [128, 3, D], F32)
        xt = [sb(f"xt{i}", [128, D], F32) for i in range(2)]
        junk = sb("junk", [128, D], BF16)
        hb = sb("hb", [128, D], BF16)
        junkb = hb
        t1 = sb("t1", [128, D], F32)
        stat = sb("stat", [128, 8], F32)
        wbuf = sb("wbuf", [128, 8, 2560], BF16)

        BIGW = 34100
        big = sb("big", [128, BIGW], F32)
        off = [0]

        def carve(words, dt=F32, shape=None):
            a = off[0]
            off[0] += words
            assert off[0] <= BIGW, off[0]
            v = big[:, a:a + words]
            if dt != F32:
                v = v.bitcast(dt)
            return v

        def reset_carve():
            off[0] = 0

        S.dma("sp", lambda e: e.dma_start(out=identb[:], in_=idb_d), writes=["identb"])
        S.dma("sp", lambda e: e.dma_start(out=identf[:], in_=idf_d), writes=["identf"])
        S.dma("sp", lambda e: e.dma_start(out=iota16[:], in_=iota_d), writes=["iota16"])
        S.op("dve", lambda e: e.tensor_scalar(out=thr16[:], in0=iota16[:], scalar1=16.0, scalar2=16.0, op0=ALU.mult, op1=ALU.add),
             reads=["iota16"], writes=["thr16"])
        S.dma("sp", lambda e: e.dma_start(out=og_bc[:], in_=og_d.partition_broadcast(128)), writes=["og_bc"])

        reset_carve()
        c2 = carve(D)
        sc2 = carve(D)
        scT = carve(16)
        scT3 = scT.rearrange("p (k b) -> p k b", k=8)
        adab2 = carve(6 * D)
        modsb = carve(6 * D)
        n1g2 = carve(D)
        n2g2 = carve(D)
        awt = [carve(8 * 512).rearrange("p (k n) -> p k n", k=8) for _ in range(2)]

        S.dma("sp", lambda e: e.dma_start(out=c2[0:2, :], in_=c_d), writes=["c2"])
        S.dma("sp", lambda e: e.dma_start(out=adab2[0:2, :], in_=adab_d.partition_broadcast(2)), writes=["adab2"])
        S.dma("sp", lambda e: e.dma_start(out=n1g2[0:2, :], in_=n1g_d.partition_broadcast(2)), writes=["n1g2"])
        S.dma("sp", lambda e: e.dma_start(out=n2g2[0:2, :], in_=n2g_d.partition_broadcast(2)), writes=["n2g2"])
        S.op("act", lambda e: e.activation(out=sc2[0:2, :], in_=c2[0:2, :], func=AF.Silu), reads=["c2"], writes=["sc2"])
        tpf = banks[5][:, 0:16].rearrange("p (k b) -> p k b", k=8)
        for k in range(8):
            S.op("pe", lambda e, k=k: e.transpose(out=tpf[:, k, :], in_=sc2[0:2, k * 128:(k + 1) * 128],
                                                  identity=identf[0:2, 0:2]),
                 reads=["sc2", "identf"], writes=["TP"])
        S.op("dve", lambda e: e.tensor_copy(out=scT3, in_=tpf), reads=["TP"], writes=["scT"])
        adaw_v = adaw_d.rearrange("(k p) n -> p k n", p=128)
        for n in range(12):
            a = awt[n % 2]
            S.dma("sp", lambda e, a=a, n=n: e.dma_start(out=a, in_=adaw_v[:, :, n * 512:(n + 1) * 512]),
                  writes=[("awt", n % 2)])
            pj = PJ[n % 2]
            for k in range(8):
                S.op("pe", lambda e, a=a, k=k, pj=pj: e.matmul(out=pj[0:2, :], lhsT=scT3[:, k, :], rhs=a[:, k, :],
                                                              start=(k == 0), stop=(k == 7)),
                     reads=["scT", ("awt", n % 2)], writes=[("PJ", n % 2)])
            S.op("dve", lambda e, pj=pj, n=n: e.tensor_tensor(out=modsb[0:2, n * 512:(n + 1) * 512], in0=pj[0:2, :],
                                                              in1=adab2[0:2, n * 512:(n + 1) * 512], op=ALU.add),
                 reads=[("PJ", n % 2), "adab2"], writes=["modsb"])
        S.op("dve", lambda e: e.scalar_tensor_tensor(out=modsb[0:2, D:2 * D], in0=modsb[0:2, D:2 * D], scalar=1.0,
                                                     in1=n1g2[0:2, :], op0=ALU.add, op1=ALU.mult),
             reads=["modsb", "n1g2"], writes=["modsb"])
        S.op("dve", lambda e: e.scalar_tensor_tensor(out=modsb[0:2, 4 * D:5 * D], in0=modsb[0:2, 4 * D:5 * D], scalar=1.0,
                                                     in1=n2g2[0:2, :], op0=ALU.add, op1=ALU.mult),
             reads=["modsb", "n2g2"], writes=["modsb"])
        for dst, src in enumerate([1, 0, 2, 4, 3, 5]):
            S.dma("sp", lambda e, dst=dst, src=src: e.dma_start(out=mods_d[:, dst, :], in_=modsb[0:2, src * D:(src + 1) * D]),
                  reads=["modsb"], writes=["modscr"])
        S.barrier()

        def norm_mod(xin, xkey, gm, sh, mkey, out_bf, out_key, out_f32=None, out_f32_key=None):
            S.op("act", lambda e: e.activation(out=junk[:], in_=xin, func=AF.Square, accum_out=stat[:, 0:1]),
                 reads=[xkey], writes=["junk", "stat"])
            S.op("dve", lambda e: e.tensor_scalar(out=stat[:, 1:2], in0=stat[:, 0:1], scalar1=1.0 / D, scalar2=EPS,
                                                  op0=ALU.mult, op1=ALU.add), reads=["stat"], writes=["stat"])
            S.op("act", lambda e: e.sqrt(out=stat[:, 3:4], in_=stat[:, 1:2]), reads=["stat"], writes=["stat"])
            S.op("dve", lambda e: e.reciprocal(out=stat[:, 2:3], in_=stat[:, 3:4]), reads=["stat"], writes=["stat"])
            S.op("dve", lambda e: e.scalar_tensor_tensor(out=t1[:], in0=xin, scalar=stat[:, 2:3], in1=gm,
                                                         op0=ALU.mult, op1=ALU.mult),
                 reads=[xkey, "stat", mkey], writes=["t1"])
            if out_f32 is not None:
                S.op("dve", lambda e: e.tensor_tensor(out=out_f32, in0=t1[:], in1=sh, op=ALU.add),
                     reads=["t1", ("modbc", 1)], writes=[out_f32_key])
                S.op("dve", lambda e: e.tensor_copy(out=out_bf, in_=out_f32), reads=[out_f32_key], writes=[out_key])
            else:
                S.op("dve", lambda e: e.tensor_tensor(out=out_bf, in0=t1[:], in1=sh, op=ALU.add),
                     reads=["t1", ("modbc", 1)], writes=[out_key])

        reset_carve()
        hT = carve(8 * SEQ // 2, BF16).rearrange("p (k t) -> p k t", k=8)
        KT = carve(4 * SEQ // 2, BF16).rearrange("p (k t) -> p k t", k=4)
        Vt = carve(NT * 8 * 66 // 2, BF16).rearrange("p (i h d) -> p i h d", i=NT, h=8)
        QT = carve(2 * 4 * 512 // 2, BF16).rearrange("p (s k t) -> p s k t", s=2, k=4)
        onTa = carve(4 * SEQ // 2, BF16).rearrange("p (k t) -> p k t", k=4)
        onTd = carve(4 * 128 // 2, BF16).rearrange("p (k t) -> p k t", k=4)
        ETs = [carve(4 * 128 // 2, BF16).rearrange("p (c t) -> p c t", c=4) for _ in range(4)]
        otile = carve(512)
        onb = carve(512 // 2, BF16)
        orec = carve(8)
        rpbT = carve(8 * 7 * 128 // 2, BF16).rearrange("p (h d q) -> p h d q", h=8, d=7)
        nam = carve(NM * 128 // 2, BF16).rearrange("p (m q) -> p m q", m=NM)
        logm = carve(17 * 2 * 128 // 2, BF16).rearrange("p (d s q) -> p d s q", d=17, s=2)
        absl = carve(3 * 128 // 2, BF16).rearrange("p (d q) -> p d q", d=3)
        absc = carve(9 * 128 // 2, BF16).rearrange("p (d q) -> p d q", d=9)
        nsI = carve(8 * 128 // 2, BF16).rearrange("p (h q) -> p h q", h=8)

        print("attn carve words", off[0], flush=True)
        cstg = [carve(2 * D // 2, BF16) for _ in range(2)]
        cblk = [0]

        def conv_step(n):
            for _ in range(n):
                blk = cblk[0]
                if blk >= 128:
                    return
                cblk[0] += 1
                r2 = blk % 2
                rows = slice(blk * 128, (blk + 1) * 128)
                S.dma("pool", lambda e, r2=r2, rows=rows: e.dma_start(out=cstg[r2][:, 0:D], in_=u_d[rows, :]), writes=[("cstg", r2, 0)])
                S.dma("pool", lambda e, r2=r2, rows=rows: e.dma_start(out=cstg[r2][:, D:2 * D], in_=v_d[rows, :]), writes=[("cstg", r2, 1)])
                S.dma("pool", lambda e, r2=r2, rows=rows: e.dma_start(out=uv_d[rows, :], in_=cstg[r2]),
                      reads=[("cstg", r2, 0), ("cstg", r2, 1)], writes=["uvtab"])

        S.dma("sp", lambda e: e.dma_start(out=rpbT, in_=rpbT_d), writes=["rpbT"])
        S.dma("sp", lambda e: e.dma_start(out=nam, in_=nam_d), writes=["nam"])
        S.dma("sp", lambda e: e.dma_start(out=logm, in_=logm_d), writes=["logm"])
        S.dma("sp", lambda e: e.dma_start(out=absl, in_=absl_d), writes=["absl"])
        S.dma("sp", lambda e: e.dma_start(out=absc, in_=absc_d), writes=["absc"])
        S.dma("sp", lambda e: e.dma_start(out=nsI, in_=nsI_d), writes=["nsI"])
        S.op("dve", lambda e: e.memset(Vt[:, :, :, 64:66], 1.0), writes=["Vones"])
        S.op("dve", lambda e: e.memset(QT, 0.0), writes=["QTzero"])

        win_v = win_d.rearrange("(k p) n -> p k n", p=128)
        wout_v = wout_d.rearrange("(k p) n -> p k n", p=128)
        wq_v = wq_d.rearrange("(k p) n -> p k n", p=128)

        def load_w(src_v, c0, ncols, dst0, key):
            for k in range(8):
                for cc in range(0, ncols, 512):
                    S.dma("pool", lambda e, k=k, cc=cc: e.dma_start(out=wbuf[:, k, dst0 + cc:dst0 + cc + 512],
                                                                    in_=src_v[:, k, c0 + cc:c0 + cc + 512]),
                          writes=[("W", k, (dst0 + cc) // 512)])

        evac_flip = [0]

        def evac(out_ap, in_ap, reads, writes, scale=None):
            evac_flip[0] ^= 1
            if evac_flip[0]:
                if scale is None:
                    S.op("act", lambda e: e.copy(out=out_ap, in_=in_ap), reads=reads, writes=writes)
                else:
                    S.op("act", lambda e: e.mul(out_ap, in_ap, scale), reads=reads, writes=writes)
            else:
                if scale is None:
                    S.op("dve", lambda e: e.tensor_copy(out=out_ap, in_=in_ap), reads=reads, writes=writes)
                else:
                    S.op("dve", lambda e: e.tensor_scalar(out=out_ap, in0=in_ap, scalar1=scale, scalar2=None,
                                                          op0=ALU.mult), reads=reads, writes=writes)

        pj_i = [0]

        def proj_fm(dst, dkey, wcol0, tok0, ntok, wkey, scale=None):
            p = pj_i[0] % 2
            pj_i[0] += 1
            pj = PJ[p]
            tiles = sorted(set(range(tok0 // 128, (tok0 + ntok) // 128)))
            for k in range(8):
                S.op("pe", lambda e, k=k, pj=pj: e.matmul(out=pj[:, 0:ntok], lhsT=wbuf[:, k, wcol0:wcol0 + 128],
                                                          rhs=hT[:, k, tok0:tok0 + ntok], start=(k == 0), stop=(k == 7)),
                     reads=[("W", k, wcol0 // 512)] + [("hT", i) for i in tiles], writes=[("PJ", p)])
            if isinstance(dst, list):
                for (d_ap, r0, dk) in dst:
                    evac(d_ap, pj[r0:r0 + 64, 0:ntok], [("PJ", p), "QTzero"], [dk], scale)
            else:
                evac(dst, pj[:, 0:ntok], [("PJ", p)], [dkey], scale)

        def proj_v(b_tile, wcol0, wkey):
            p = pj_i[0] % 2
            pj_i[0] += 1
            pj = PJ[p]
            for k in range(8):
                S.op("pe", lambda e, k=k, pj=pj: e.matmul(out=pj[:, :], lhsT=hT[:, k, b_tile * 128:(b_tile + 1) * 128],
                                                          rhs=wbuf[:, k, wcol0:wcol0 + 512], start=(k == 0), stop=(k == 7)),
                     reads=[("W", k, wcol0 // 512), ("hT", b_tile)], writes=[("PJ", p)])
            evac(Vt[:, b_tile, :, 0:64], pj[:, :].rearrange("p (h d) -> p h d", h=8), [("PJ", p)], [("V", b_tile)])

        sring = [0]

        def attention_tile(i, chunks, bias_for, okey, mid=None):
            iq = (i % 4) * 128
            groups = [chunks[a:a + 4] for a in range(0, len(chunks), 4)]
            units = [(h, gi) for h in range(8) for gi in range(len(groups))]
            ring = {}
            LAG = 2

            def scores(h, gi):
                fc = h // 2
                grp = groups[gi]
                bias_mms = bias_for(h)
                g = sring[0] % 3
                sring[0] += 1
                ring[(h, gi)] = g
                for ci, j in enumerate(grp):
                    S.op("pe", lambda e, g=g, ci=ci, j=j, fc=fc, h=h: e.matmul(out=Sps[g][:, ci, :],
                                                                             lhsT=KT[:, fc, j * 128:(j + 1) * 128],
                                                                             rhs=QT[:, h % 2, fc, iq:iq + 128], start=True, stop=False),
                         reads=[("KT", fc, j // 4), ("QT", fc, h % 2), "QTzero"], writes=[("S", g)])
                    bm = bias_mms(j)
                    for bi, (lt, rh, rk) in enumerate(bm):
                        S.op("pe", lambda e, g=g, ci=ci, lt=lt, rh=rh, last=(bi == len(bm) - 1):
                             e.matmul(out=Sps[g][:, ci, :], lhsT=lt, rhs=rh, start=False, stop=last),
                             reads=rk, writes=[("S", g)])
                n = len(grp)
                S.op("act", lambda e, g=g, n=n: e.activation(out=ETs[g][:, 0:n, :], in_=Sps[g][:, 0:n, :], func=AF.Exp),
                     reads=[("S", g)], writes=[("ET", g)])

            def pv(h, gi):
                grp = groups[gi]
                g = ring[(h, gi)]
                slot = h % 2
                for ci, j in enumerate(grp):
                    first = (gi == 0 and ci == 0)
                    last = (gi == len(groups) - 1 and ci == len(grp) - 1)
                    S.op("pe", lambda e, g=g, ci=ci, j=j, h=h, slot=slot, first=first, last=last: e.matmul(
                        out=Ops2[slot][:, 0:66], lhsT=ETs[g][:, ci, :], rhs=Vt[:, j, h, 0:66], start=first, stop=last),
                        reads=[("ET", g), ("V", j), "Vones"], writes=[("O", slot)])
                if gi == len(groups) - 1:
                    S.op("dve", lambda e, h=h, slot=slot: e.reciprocal(out=orec[:, h:h + 1], in_=Ops2[slot][:, 64:65]),
                         reads=[("O", slot)], writes=["orec"])
                    S.op("dve", lambda e, h=h, slot=slot: e.tensor_scalar(out=otile[:, h * 64:(h + 1) * 64], in0=Ops2[slot][:, 0:64],
                                                                          scalar1=orec[:, h:h + 1], scalar2=None, op0=ALU.mult),
                         reads=[("O", slot), "orec"], writes=[okey])

            for k in range(len(units) + LAG):
                if k < len(units):
                    scores(*units[k])
                if k == LAG and mid is not None:
                    mid()
                if k >= LAG:
                    pv(*units[k - LAG])

        def out_norm(okey, gcol0, dstT, dst_cols, dkey):
            S.op("act", lambda e: e.activation(out=junk[:, 0:512], in_=otile, func=AF.Square, accum_out=stat[:, 4:5]),
                 reads=[okey], writes=["junk", "stat2"])
            S.op("dve", lambda e: e.tensor_scalar(out=stat[:, 5:6], in0=stat[:, 4:5], scalar1=1.0 / 512, scalar2=EPS,
                                                  op0=ALU.mult, op1=ALU.add), reads=["stat2"], writes=["stat2"])
            S.op("act", lambda e: e.sqrt(out=stat[:, 7:8], in_=stat[:, 5:6]), reads=["stat2"], writes=["stat2"])
            S.op("dve", lambda e: e.reciprocal(out=stat[:, 6:7], in_=stat[:, 7:8]), reads=["stat2"], writes=["stat2"])
            S.op("dve", lambda e: e.scalar_tensor_tensor(out=onb, in0=otile, scalar=stat[:, 6:7],
                                                         in1=og_bc[:, gcol0:gcol0 + 512], op0=ALU.mult, op1=ALU.mult),
                 reads=[okey, "stat2", "og_bc"], writes=["onb"])
            for k in range(4):
                S.op("pe", lambda e, k=k: e.transpose(out=TPb[:, k, :], in_=onb[:, k * 128:(k + 1) * 128], identity=identb[:]),
                     reads=["onb", "identb"], writes=["TP"])
            evac(dstT[:, :, dst_cols], TPb[:, 0:4, :], ["TP"], [dkey])

        def na_bias(i, h):
            plan = {j: (dl, mid) for (j, dl, mid) in NA_PLAN[i]}

            def f(j):
                dl, mid = plan[j]
                return [(identb[:], rpbT[:, h, dl + 3, :], ["identb", "rpbT"]),
                        (identb[:], nam[:, mid, :], ["identb", "nam"])]
            return f

        def dil_bias(i, h):
            def f(j):
                dl = j - i
                sg = 0 if dl > 0 else (1 if dl < 0 else 2)
                mm = [(nsI[:, h, :], absl[:, sg, :], ["nsI", "absl"])]
                if dl != 0:
                    mm.append((nsI[:, h, :], absc[:, abs(dl), :], ["nsI", "absc"]))
                mm.append((identb[:], logm[:, dl + 8, 0, :], ["identb", "logm"]))
                if abs(dl) <= 2:
                    mm.append((identb[:], logm[:, dl + 8, 1, :], ["identb", "logm"]))
                return mm
            return f

        xcount = [0]
        epi = [None]
        for b in range(NB):
            for vi in range(3):
                S.dma("sp", lambda e, b=b, vi=vi: e.dma_start(out=modbc[:, vi, :], in_=mods_d[b, vi, :].partition_broadcast(128)),
                      reads=["modscr"], writes=[("modbc", vi)])
            for i in range(NT):
                xb = xt[xcount[0] % 2]
                xk = ("xt", xcount[0] % 2)
                xcount[0] += 1
                S.dma("sp", lambda e, b=b, xb=xb, i=i: e.dma_start(out=xb[:], in_=x_d[b, i * 128:(i + 1) * 128, :]), writes=[xk])
                norm_mod(xb[:], xk, modbc[:, 0, :], modbc[:, 1, :], ("modbc", 0), hb[:], "hb")
                for k in range(8):
                    S.op("pe", lambda e, k=k: e.transpose(out=TPb[:, k, :], in_=hb[:, k * 128:(k + 1) * 128], identity=identb[:]),
                         reads=["hb", "identb"], writes=["TP"])
                evac(hT[:, :, i * 128:(i + 1) * 128], TPb[:, :, :], ["TP"], [("hT", i)])
            load_w(win_v, 0, 1536, 0, "wA")
            for fc in range(4):
                for g4 in range(4):
                    proj_fm(KT[:, fc, g4 * 512:(g4 + 1) * 512], ("KT", fc, g4), 512 + fc * 128, g4 * 512, 512, "wA")
            for i in range(NT):
                proj_v(i, 1024, "wA")
            for i in range(NT):
                if i % 4 == 0:
                    for fc in range(4):
                        proj_fm([(QT[0:64, 0, fc, :], 0, ("QT", fc, 0)), (QT[64:128, 1, fc, :], 64, ("QT", fc, 1))], None, fc * 128, i * 128, 512, "wA", scale=0.125)
                conv_step(2)
                attention_tile(i, [j for (j, _, _) in NA_PLAN[i]], lambda h, i=i: na_bias(i, h), "otile", mid=epi[0])
                epi[0] = (lambda i=i: out_norm("otile", 0, onTa, slice(i * 128, (i + 1) * 128), ("onTa", i)))
            epi[0]()
            epi[0] = None
            load_w(win_v, 1536, 1536, 0, "wA")
            load_w(wout_v, 0, 1024, 1536, "wO")
            for fc in range(4):
                for g4 in range(4):
                    proj_fm(KT[:, fc, g4 * 512:(g4 + 1) * 512], ("KT", fc, g4), 512 + fc * 128, g4 * 512, 512, "wA")
            for i in range(NT):
                proj_v(i, 1024, "wA")
            for i in range(NT):
                if i % 4 == 0:
                    for fc in range(4):
                        proj_fm([(QT[0:64, 0, fc, :], 0, ("QT", fc, 0)), (QT[64:128, 1, fc, :], 64, ("QT", fc, 1))], None, fc * 128, i * 128, 512, "wA", scale=0.125)
                conv_step(2)
                chunks = [j for j in range(NT) if abs(j - i) <= 8]
                attention_tile(i, chunks, lambda h, i=i: dil_bias(i, h), "otile", mid=epi[0])

                def _epi(b=b, i=i):
                    out_norm("otile", 512, onTd, slice(0, 128), "onTd")
                    for half in range(2):
                        pj = PJ[half]
                        for k in range(8):
                            lt = onTa[:, k, i * 128:(i + 1) * 128] if k < 4 else onTd[:, k - 4, :]
                            rk = [("onTa", i)] if k < 4 else ["onTd"]
                            S.op("pe", lambda e, k=k, pj=pj, lt=lt, half=half: e.matmul(
                                out=pj[:, :], lhsT=lt, rhs=wbuf[:, k, 1536 + half * 512:1536 + (half + 1) * 512],
                                start=(k == 0), stop=(k == 7)), reads=rk + [("W", k, 3 + half)], writes=[("PJ", half)])
                    xb = xt[xcount[0] % 2]
                    xk = ("xt", xcount[0] % 2)
                    xcount[0] += 1
                    S.dma("sp", lambda e, b=b, xb=xb, i=i: e.dma_start(out=xb[:], in_=x_d[b, i * 128:(i + 1) * 128, :]), writes=[xk])
                    for half in range(2):
                        S.op("dve", lambda e, half=half: e.tensor_tensor(out=t1[:, half * 512:(half + 1) * 512], in0=PJ[half][:, :],
                                                                          in1=modbc[:, 2, half * 512:(half + 1) * 512], op=ALU.mult),
                             reads=[("PJ", half), ("modbc", 2)], writes=["t1"])
                    S.op("dve", lambda e, xb=xb: e.tensor_tensor(out=xb[:], in0=xb[:], in1=t1[:], op=ALU.add),
                         reads=[xk, "t1"], writes=[xk])
                    S.dma("sp", lambda e, b=b, xb=xb, i=i: e.dma_start(out=x1_d[b, i * 128:(i + 1) * 128, :], in_=xb[:]),
                          reads=[xk], writes=[("x1s", b, i)], is_output=debug)

                epi[0] = _epi
            epi[0]()
            epi[0] = None
        conv_step(128)
        load_w(wq_v, 0, 2048, 0, "wQ")
        S.barrier()

        reset_carve()
        h2b = [carve(D // 2, BF16) for _ in range(2)]
        h2T = carve(8 * 128 // 2, BF16).rearrange("p (k t) -> p k t", k=8)
        qTs = carve(16 * 128).rearrange("p (c t) -> p c t", c=16)
        skT = carve(2 * 128).rearrange("p (s n) -> p s n", s=2)
        sks = carve(2 * 128).rearrange("p (s n) -> p s n", s=2)
        scs = carve(2048).rearrange("p (g n) -> p g n", g=16)
        sc2_ = carve(128)
        tv = carve(256).rearrange("p (g k) -> p g k", g=16)
        ti = carve(256, U32).rearrange("p (g k) -> p g k", g=16)
        tif = carve(256).rearrange("p (h s k) -> p h s k", h=8, s=2)
        cand = carve(2048).rearrange("p (h a b) -> p h a b", h=8, a=16)
        cand2 = carve(256)
        bv = carve(128).rearrange("p (h k) -> p h k", h=8)
        bp = carve(128, U32).rearrange("p (h k) -> p h k", h=8)
        kif = carve(128).rearrange("p (h k) -> p h k", h=8)
        kjf = carve(128).rearrange("p (h k) -> p h k", h=8)
        eq = cand
        If = carve(128).rearrange("p (h k) -> p h k", h=8)
        Jf = carve(128).rearrange("p (h k) -> p h k", h=8)
        idsf = carve(128)
        ids2 = [carve(128, U32) for _ in range(2)]
        gsum = carve(8)
        gates2 = [carve(128).rearrange("p (h k) -> p h k", h=8) for _ in range(2)]
        adot = carve(128)
        gl = carve(128)
        wg = carve(128)
        NSLOT = 16
        uvg = [carve(2 * D // 2, BF16) for _ in range(NSLOT)]
        prod = [carve(D // 2, BF16) for _ in range(4)]
        dg = [carve(64, BF16) for _ in range(8)]
        modbcP = [modbc, carve(3 * D).rearrange("p (v d) -> p v d", v=3)]

        QP = banks[0][:].rearrange("p (c t) -> p c t", c=4)
        SCP = [banks[1 + q][:].rearrange("p (c t) -> p c t", c=4) for q in range(4)]
        YP = [banks[6], banks[7]]

        S.dma("sp", lambda e: e.dma_start(out=fg_bc[:], in_=fg_d.partition_broadcast(128)), writes=["fg_bc"])
        S.dma("sp", lambda e: e.dma_start(out=sks, in_=sk_d.rearrange("s n d -> n s d")), writes=["sks"])
        tpf2 = banks[5][:, 0:256].rearrange("p (s n) -> p s n", s=2)
        for s_ in range(2):
            S.op("pe", lambda e, s_=s_: e.transpose(out=tpf2[:, s_, :], in_=sks[:, s_, :], identity=identf[:]),
                 reads=["sks", "identf"], writes=["TP"])
        S.op("dve", lambda e: e.tensor_copy(out=skT, in_=tpf2), reads=["TP"], writes=["skT"])
        for b in range(NB):
            for vi in range(3):
                S.dma("sp", lambda e, b=b, vi=vi: e.dma_start(out=modbcP[b][:, vi, :], in_=mods_d[b, 3 + vi, :].partition_broadcast(128)),
                      writes=[("modbcP", b, vi)])

        def peer_front(b, i, par):
            xb = xt[par]
            xk = ("xt", par)
            ids = ids2[par]
            gates = gates2[par]
            hbk = ("h2b", par)
            mb = modbcP[b]
            S.dma("sp", lambda e: e.dma_start(out=xb[:], in_=x1_d[b, i * 128:(i + 1) * 128, :]), writes=[xk])
            S.op("act", lambda e: e.activation(out=junk[:], in_=xb[:], func=AF.Square, accum_out=stat[:, 0:1]),
                 reads=[xk], writes=["junk", "stat"])
            S.op("dve", lambda e: e.tensor_scalar(out=stat[:, 1:2], in0=stat[:, 0:1], scalar1=1.0 / D, scalar2=EPS,
                                                  op0=ALU.mult, op1=ALU.add), reads=["stat"], writes=["stat"])
            S.op("act", lambda e: e.sqrt(out=stat[:, 3:4], in_=stat[:, 1:2]), reads=["stat"], writes=["stat"])
            S.op("dve", lambda e: e.reciprocal(out=stat[:, 2:3], in_=stat[:, 3:4]), reads=["stat"], writes=["stat"])
            S.op("dve", lambda e: e.scalar_tensor_tensor(out=t1[:], in0=xb[:], scalar=stat[:, 2:3], in1=mb[:, 0, :],
                                                         op0=ALU.mult, op1=ALU.mult),
                 reads=[xk, "stat", ("modbcP", b, 0)], writes=["t1"])
            S.op("dve", lambda e: e.tensor_tensor(out=h2b[par], in0=t1[:], in1=mb[:, 1, :], op=ALU.add),
                 reads=["t1", ("modbcP", b, 1)], writes=[hbk])
            for k in range(8):
                S.op("pe", lambda e, k=k: e.transpose(out=TPb[:, k, :], in_=h2b[par][:, k * 128:(k + 1) * 128], identity=identb[:]),
                     reads=[hbk, "identb"], writes=["TP"])
            evac(h2T, TPb[:, :, :], ["TP"], ["h2T"])
            for c4 in range(4):
                for cc in range(4):
                    c = c4 * 4 + cc
                    for k in range(8):
                        S.op("pe", lambda e, c=c, cc=cc, k=k: e.matmul(out=QP[:, cc, :], lhsT=wbuf[:, k, c * 128:(c + 1) * 128],
                                                                       rhs=h2T[:, k, :], start=(k == 0), stop=(k == 7)),
                             reads=[("W", k, c // 4), "h2T"], writes=["QP"])
                evac(qTs[:, c4 * 4:(c4 + 1) * 4, :], QP, ["QP"], [("qTs", c4)])
            for q4 in range(4):
                for cc in range(4):
                    c = q4 * 4 + cc
                    S.op("pe", lambda e, c=c, cc=cc, q4=q4: e.matmul(out=SCP[q4][:, cc, :], lhsT=qTs[:, c, :], rhs=skT[:, c % 2, :],
                                                                     start=True, stop=True),
                         reads=[("qTs", q4), "skT"], writes=[("SCP", q4)])
                S.op("act", lambda e, q4=q4: e.copy(out=scs[:, q4 * 4:(q4 + 1) * 4, :], in_=SCP[q4]),
                     reads=[("SCP", q4)], writes=[("scs", q4)])
            for g in range(16):
                rk = [("scs", g // 4)]
                S.op("dve", lambda e, g=g: e.max(out=tv[:, g, 0:8], in_=scs[:, g, :]), reads=rk, writes=["tv"])
                S.op("dve", lambda e, g=g: e.max_index(out=ti[:, g, 0:8], in_max=tv[:, g, 0:8], in_values=scs[:, g, :]),
                     reads=rk + ["tv"], writes=["ti"])
                S.op("dve", lambda e, g=g: e.match_replace(out=sc2_, in_to_replace=tv[:, g, 0:8], in_values=scs[:, g, :],
                                                           imm_value=-1e30), reads=rk + ["tv"], writes=["sc2_"])
                S.op("dve", lambda e, g=g: e.max(out=tv[:, g, 8:16], in_=sc2_), reads=["sc2_"], writes=["tv"])
                S.op("dve", lambda e, g=g: e.max_index(out=ti[:, g, 8:16], in_max=tv[:, g, 8:16], in_values=sc2_),
                     reads=["sc2_", "tv"], writes=["ti"])
            S.op("dve", lambda e: e.tensor_copy(out=tif.rearrange("p h s k -> p (h s k)"),
                                                in_=ti.rearrange("p g k -> p (g k)")), reads=["ti"], writes=["tif"])
            tvv = tv.rearrange("p (h s) k -> p h s k", s=2)
            S.op("dve", lambda e: e.tensor_tensor(out=cand, in0=tvv[:, :, 0, :].unsqueeze(3).to_broadcast([128, 8, 16, 16]),
                                                  in1=tvv[:, :, 1, :].unsqueeze(2).to_broadcast([128, 8, 16, 16]), op=ALU.add),
                 reads=["tv"], writes=["cand"], cost=2.3)
            for h in range(8):
                ch = cand[:, h, :, :].rearrange("p a b -> p (a b)")
                S.op("dve", lambda e, h=h, ch=ch: e.max(out=bv[:, h, 0:8], in_=ch), reads=["cand"], writes=["bv"])
                S.op("dve", lambda e, h=h, ch=ch: e.max_index(out=bp[:, h, 0:8], in_max=bv[:, h, 0:8], in_values=ch),
                     reads=["cand", "bv"], writes=["bp"])
                S.op("dve", lambda e, h=h, ch=ch: e.match_replace(out=cand2, in_to_replace=bv[:, h, 0:8], in_values=ch,
                                                                  imm_value=-1e30), reads=["cand", "bv"], writes=["cand2"])
                S.op("dve", lambda e, h=h: e.max(out=bv[:, h, 8:16], in_=cand2), reads=["cand2"], writes=["bv"])
                S.op("dve", lambda e, h=h: e.max_index(out=bp[:, h, 8:16], in_max=bv[:, h, 8:16], in_values=cand2),
                     reads=["cand2", "bv"], writes=["bp"])
            S.op("dve", lambda e: e.tensor_copy(out=If, in_=bp), reads=["bp"], writes=["If"])
            thb = thr16[:].unsqueeze(1).unsqueeze(1).to_broadcast([128, 8, 16, 16])
            S.op("dve", lambda e: e.tensor_tensor(out=eq, in0=If.unsqueeze(3).to_broadcast([128, 8, 16, 16]), in1=thb, op=ALU.is_ge),
                 reads=["If", "thr16"], writes=["eq"], cost=2.3)
            S.op("dve", lambda e: e.tensor_reduce(out=kif, in_=eq, axis=AX.X, op=ALU.add), reads=["eq"], writes=["kif"], cost=2.3)
            S.op("dve", lambda e: e.scalar_tensor_tensor(out=kjf, in0=kif, scalar=-16.0, in1=If, op0=ALU.mult, op1=ALU.add),
                 reads=["kif", "If"], writes=["kjf"])
            iob = iota16[:].unsqueeze(1).unsqueeze(1).to_broadcast([128, 8, 16, 16])
            for (kf, kkey, sidx, dst, dkey) in ((kif, "kif", 0, If, "If"), (kjf, "kjf", 1, Jf, "Jf")):
                S.op("dve", lambda e, kf=kf: e.tensor_tensor(out=eq, in0=kf.unsqueeze(3).to_broadcast([128, 8, 16, 16]),
                                                             in1=iob, op=ALU.is_equal), reads=[kkey, "iota16"], writes=["eq"], cost=2.3)
                S.op("dve", lambda e, sidx=sidx: e.tensor_tensor(out=eq, in0=eq,
                                                                 in1=tif[:, :, sidx, :].unsqueeze(2).to_broadcast([128, 8, 16, 16]),
                                                                 op=ALU.mult), reads=["eq", "tif"], writes=["eq"], cost=2.3)
                S.op("dve", lambda e, dst=dst: e.tensor_reduce(out=dst, in_=eq, axis=AX.X, op=ALU.add),
                     reads=["eq", "kjf"], writes=[dkey], cost=2.3)
            S.op("dve", lambda e: e.scalar_tensor_tensor(out=idsf, in0=If.rearrange("p h k -> p (h k)"), scalar=128.0,
                                                         in1=Jf.rearrange("p h k -> p (h k)"), op0=ALU.mult, op1=ALU.add),
                 reads=["If", "Jf"], writes=["idsf"])
            S.op("dve", lambda e: e.tensor_scalar(out=idsf, in0=idsf, scalar1=16383.0, scalar2=0.0, op0=ALU.min, op1=ALU.max),
                 reads=["idsf"], writes=["idsf"])
            S.op("dve", lambda e: e.tensor_copy(out=ids, in_=idsf), reads=["idsf"], writes=[("ids", par)])
            gk = ("gates", par)
            S.op("dve", lambda e: e.tensor_tensor(out=gates, in0=bv, in1=bv[:, :, 0:1].to_broadcast([128, 8, 16]), op=ALU.subtract),
                 reads=["bv"], writes=[gk])
            S.op("act", lambda e: e.activation(out=gates, in_=gates, func=AF.Exp), reads=[gk], writes=[gk])
            S.op("dve", lambda e: e.tensor_reduce(out=gsum, in_=gates, axis=AX.X, op=ALU.add), reads=[gk], writes=["gsum"])
            S.op("dve", lambda e: e.reciprocal(out=gsum, in_=gsum), reads=["gsum"], writes=["gsum"])
            S.op("dve", lambda e: e.tensor_tensor(out=gates, in0=gates, in1=gsum.unsqueeze(2).to_broadcast([128, 8, 16]), op=ALU.mult),
                 reads=[gk, "gsum"], writes=[gk])

        tiles = [(b, i) for b in range(NB) for i in range(NT)]
        S.defer = []
        peer_front(tiles[0][0], tiles[0][1], 0)
        pending = S.defer
        S.defer = None
        S.run_deferred(pending, len(pending))
        gcount = [0]
        pcount = [0]
        print('peer carve words', off[0], flush=True)
        for ti_, (b, i) in enumerate(tiles):
            par = ti_ % 2
            nxt = []
            if ti_ + 1 < len(tiles):
                S.defer = []
                peer_front(tiles[ti_ + 1][0], tiles[ti_ + 1][1], 1 - par)
                nxt = S.defer
                S.defer = None
            per_slot = (len(nxt) + 111) // 112
            budget = sum(t[3] for t in nxt) / 27.0 if nxt else 0.0
            ids = ids2[par]
            gates = gates2[par].rearrange("p h k -> p (h k)")
            GB = 4

            def gathers(e0):
                sls = []
                for e_ in range(e0, e0 + GB):
                    sl = gcount[0] % NSLOT
                    gcount[0] += 1
                    sls.append(sl)
                    S.dma("pool", lambda e, e_=e_, sl=sl, ids=ids: e.indirect_dma_start(
                        out=uvg[sl], out_offset=None, in_=uv_d, in_offset=bass.IndirectOffsetOnAxis(ap=ids[:, e_:e_ + 1], axis=0)),
                        reads=[("ids", par)], writes=[("uvg", sl)])
                return sls

            def dots(e0, sls):
                for n_, e_ in enumerate(range(e0, e0 + GB)):
                    sl = sls[n_]
                    ak = ("adot", (e_ // GB) % 2, e_ % 4)
                    pr = pcount[0] % 4
                    pcount[0] += 1
                    S.op("dve", lambda e, sl=sl, par=par, pr=pr: e.tensor_tensor(out=prod[pr], in0=uvg[sl][:, 0:D], in1=h2b[par], op=ALU.mult),
                         reads=[("uvg", sl), ("h2b", par)], writes=[("prod", pr)])
                    S.op("act", lambda e, e_=e_, pr=pr: e.activation(out=junk[:], in_=prod[pr], func=AF.Copy, accum_out=adot[:, e_:e_ + 1]),
                         reads=[("prod", pr)], writes=["junk", ak])

            def gelu_stage(e0, sls):
                gb = (e0 // GB) % 2
                S.op("act", lambda e, e0=e0: e.activation(out=gl[:, e0:e0 + GB], in_=adot[:, e0:e0 + GB], func=AF.Gelu),
                     reads=[("adot", gb, 0), ("adot", gb, 1), ("adot", gb, 2), ("adot", gb, 3)], writes=[("gl", gb)])

            def combine(e0, sls):
                gb = (e0 // GB) % 2
                S.op("dve", lambda e, e0=e0, gates=gates: e.tensor_tensor(out=wg[:, e0 + 2:e0 + 4], in0=gl[:, e0 + 2:e0 + 4],
                                                                           in1=gates[:, e0 + 2:e0 + 4], op=ALU.mult),
                     reads=[("gl", gb), ("gates", par)], writes=[("wg", gb)])
                for n_, e_ in enumerate(range(e0, e0 + GB)):
                    sl = sls[n_]
                    d_ = e_ % 8
                    if n_ < 2:
                        S.op("dve", lambda e, e_=e_, d_=d_, gates=gates: e.tensor_scalar(out=dg[d_], in0=identb[:], scalar1=gl[:, e_:e_ + 1],
                                                                                         scalar2=gates[:, e_:e_ + 1], op0=ALU.mult, op1=ALU.mult),
                             reads=[("gl", gb), ("gates", par), "identb"], writes=[("dg", d_)])
                    else:
                        S.op("act", lambda e, e_=e_, d_=d_: e.activation(out=dg[d_], in_=identb[:], func=AF.Copy, scale=wg[:, e_:e_ + 1]),
                             reads=[("wg", gb), "identb"], writes=[("dg", d_)])
                    for half in range(2):
                        S.op("pe", lambda e, e_=e_, sl=sl, d_=d_, half=half: e.matmul(
                            out=YP[half][:, :], lhsT=dg[d_], rhs=uvg[sl][:, D + half * 512:D + (half + 1) * 512],
                            start=(e_ == 0), stop=(e_ == 127)), reads=[("dg", d_), ("uvg", sl)], writes=[("YP", half)])

            prev = None
            for e0 in range(0, 128, GB):
                sls = gathers(e0)
                if prev is not None:
                    gelu_stage(*prev)
                dots(e0, sls)
                if prev is not None:
                    combine(*prev)
                prev = (e0, sls)
                if e0 >= 8:
                    S.run_deferred_budget(nxt, budget)
            gelu_stage(*prev)
            combine(*prev)
            S.run_deferred(nxt, len(nxt))
            xb = xt[par]
            xk = ("xt", par)
            mb = modbcP[b]
            for half in range(2):
                S.op("dve", lambda e, half=half, mb=mb: e.tensor_tensor(out=t1[:, half * 512:(half + 1) * 512], in0=YP[half][:, :],
                                                                         in1=mb[:, 2, half * 512:(half + 1) * 512], op=ALU.mult),
                     reads=[("YP", half), ("modbcP", b, 2)], writes=["t1"])
            S.op("dve", lambda e, xb=xb: e.tensor_tensor(out=xb[:], in0=xb[:], in1=t1[:], op=ALU.add),
                 reads=[xk, "t1"], writes=[xk])
            S.op("act", lambda e, xb=xb: e.activation(out=junk[:], in_=xb[:], func=AF.Square, accum_out=stat[:, 4:5]),
                 reads=[xk], writes=["junk", "stat2"])
            S.op("dve", lambda e: e.tensor_scalar(out=stat[:, 5:6], in0=stat[:, 4:5], scalar1=1.0 / D, scalar2=EPS,
                                                  op0=ALU.mult, op1=ALU.add), reads=["stat2"], writes=["stat2"])
            S.op("act", lambda e: e.sqrt(out=stat[:, 7:8], in_=stat[:, 5:6]), reads=["stat2"], writes=["stat2"])
            S.op("dve", lambda e: e.reciprocal(out=stat[:, 6:7], in_=stat[:, 7:8]), reads=["stat2"], writes=["stat2"])
            S.op("dve", lambda e, xb=xb: e.scalar_tensor_tensor(out=xb[:], in0=xb[:], scalar=stat[:, 6:7], in1=fg_bc[:],
                                                                op0=ALU.mult, op1=ALU.mult),
                 reads=[xk, "stat2", "fg_bc"], writes=[xk])
            S.dma("sp", lambda e, b=b, i=i, xb=xb: e.dma_start(out=out_d[b, i * 128:(i + 1) * 128, :], in_=xb[:]),
                  reads=[xk], writes=[("out", b, i)], is_output=True)
        S.finish()
        S.emit()
        print("ops per engine:", {k: len(v) for k, v in S.ops.items()}, "sems:", S.nsem, flush=True)
    return nc


_CONSTS = None


def _consts(na_rpb):
    global _CONSTS
    if _CONSTS is None:
        logm, absl, absc, nsI = _dil_tables()
        dr, dc = _na_rpb_index()
        _CONSTS = dict(
            na_masks=_bf(NA_MASKS.transpose(1, 0, 2)),
            logm=_bf(logm.transpose(2, 0, 1, 3)),
            absl=_bf(absl.transpose(1, 0, 2)),
            absc=_bf(absc.transpose(1, 0, 2)),
            nsI=_bf(nsI.transpose(1, 0, 2)),
            identb=_bf(np.eye(128, dtype=np.float32)),
            identf=np.eye(128, dtype=np.float32),
            iota16=np.ascontiguousarray(np.broadcast_to(np.arange(16, dtype=np.float32), (128, 16))),
            _dr=dr, _dc=dc,
        )
    cst = {k: v for k, v in _CONSTS.items() if not k.startswith("_")}
    rp = np.asarray(na_rpb, np.float32)[0]
    g = rp[:, _CONSTS["_dr"], _CONSTS["_dc"]]
    cst["rpbT"] = _bf(g.transpose(2, 0, 1, 3))
    return cst


def kernel(x, c, ada_w, ada_b, norm1_g, w_in, na_rpb, out_norm_na_g, out_norm_dil_g, w_out, norm2_g,
           peer_wq, peer_subkeys, peer_u, peer_v, final_g, _debug=False, _cores=None):
    f = lambda a: np.ascontiguousarray(np.asarray(a, dtype=np.float32))
    x = f(x); c = f(c)
    shared = dict(
        ada_w=f(ada_w)[0], ada_b=f(ada_b)[0], norm1_g=f(norm1_g)[0], norm2_g=f(norm2_g)[0], final_g=f(final_g),
        og=np.ascontiguousarray(np.concatenate([f(out_norm_na_g)[0], f(out_norm_dil_g)[0]])),
        w_in=f(w_in)[0], w_out=f(w_out)[0], peer_wq=f(peer_wq)[0], peer_subkeys=f(peer_subkeys)[0],
        peer_u=f(peer_u)[0], peer_v=f(peer_v)[0],
    )
    shared.update(_consts(na_rpb))
    cores = list(range(NCORES)) if _cores is None else _cores
    nc = build_nc(debug=_debug)
    in_maps = []
    for ci in cores:
        m = dict(shared)
        m["x"] = np.ascontiguousarray(x[ci * NB:(ci + 1) * NB])
        m["c"] = np.ascontiguousarray(c[ci * NB:(ci + 1) * NB])
        in_maps.append(m)
    res = run_bass_kernel_spmd(nc, in_maps, core_ids=list(range(len(cores))))
    out = np.concatenate([np.asarray(r["out"]) for r in res.results], axis=0).astype(np.float32)
    if _debug:
        return out, [r for r in res.results]
    return out
```

```python
import numpy as np
from contextlib import ExitStack
import ml_dtypes
import concourse.bass as bass
import concourse.mybir as mybir
from concourse.bass_utils import run_bass_kernel_spmd

F32 = mybir.dt.float32
BF16 = mybir.dt.bfloat16
U32 = mybir.dt.uint32
AF = mybir.ActivationFunctionType
ALU = mybir.AluOpType
AX = mybir.AxisListType

D = 1024
SEQ = 2048
NT = SEQ // 128
NB = 2
NCORES = 8
EPS = 1e-6
NEG = -32768.0

ENG = ("pe", "act", "dve", "pool", "sp")
EPOCH = 30000
NDMA_SLOTS = 17


class Sched:
    def __init__(self, nc, stack):
        self.nc = nc
        self.stack = stack
        self.ops = {e: [] for e in ENG}
        self.cnt = {e: 0 for e in ENG}
        self.sem = {}
        self.nsem = 0
        self.eng_sem_ids = set()
        for e in ENG:
            self.sem[e] = self._newsem(e)
            self.eng_sem_ids.add(id(self.sem[e]))
        self.dma_sems = {}
        self.dma_n = {}
        self.last_w = {}
        self.readers = {}
        self.seen = {e: {} for e in ENG}
        self.out_tokens = []
        self.latest = {}
        self.defer = None

    def _newsem(self, name):
        self.nsem += 1
        return self.stack.enter_context(self.nc.semaphore(f"s{self.nsem}_{name}"))

    def _deps(self, eng, reads, writes, extra=()):
        deps = list(extra)
        for k in reads:
            t = self.last_w.get(k)
            if t is not None:
                deps.append(t)
        for k in writes:
            t = self.last_w.get(k)
            if t is not None and (t[2] != eng or t[3]):
                deps.append(t)
            for t in self.readers.get(k, ()):
                if t[2] != eng or t[3]:
                    deps.append(t)
        waits = {}
        seen = self.seen[eng]
        for (s, v, e, isdma) in deps:
            if e == eng and eng == "pe" and not isdma:
                continue
            if seen.get(id(s), 0) >= v:
                continue
            if waits.get(id(s), (None, 0))[1] < v:
                waits[id(s)] = (s, v)
        for (s, v) in waits.values():
            seen[id(s)] = v
        return list(waits.values())

    def _commit(self, tok, reads, writes):
        for k in writes:
            self.last_w[k] = tok
            self.readers[k] = []
        for k in reads:
            self.readers.setdefault(k, []).append(tok)
        self.latest[(tok[2], id(tok[0]))] = tok

    def run_deferred(self, lst, n):
        for _ in range(min(n, len(lst))):
            kind, args, kw, cost = lst.pop(0)
            getattr(self, kind)(*args, **kw)

    def run_deferred_budget(self, lst, budget):
        acc = 0.0
        while lst and acc < budget:
            kind, args, kw, cost = lst.pop(0)
            getattr(self, kind)(*args, **kw)
            acc += cost

    def op(self, eng, emit, reads=(), writes=(), extra=(), cost=None):
        if self.defer is not None:
            if cost is None:
                cost = 0.35 if eng == "dve" else 0.0
            self.defer.append(("op", (eng, emit, reads, writes), {}, cost))
            return None
        waits = self._deps(eng, reads, writes, extra)
        if self.cnt[eng] >= EPOCH:
            self.sem[eng] = self._newsem(eng)
            self.eng_sem_ids.add(id(self.sem[eng]))
            self.cnt[eng] = 0
        self.cnt[eng] += 1
        tok = (self.sem[eng], self.cnt[eng], eng, False)
        self.ops[eng].append((waits, emit, tok[0], -self.cnt[eng]))
        self._commit(tok, reads, writes)
        return tok

    def dma(self, eng, emit, reads=(), writes=(), is_output=False, extra=()):
        if self.defer is not None:
            self.defer.append(("dma", (eng, emit, reads, writes, is_output), {}, 0.0))
            return None
        waits = self._deps(eng, reads, writes, extra)
        if eng not in self.dma_sems:
            self.dma_sems[eng] = [self._newsem(f"dma_{eng}{i}") for i in range(NDMA_SLOTS)]
            self.dma_n[eng] = 0
        n = self.dma_n[eng]
        self.dma_n[eng] += 1
        s = self.dma_sems[eng][n % NDMA_SLOTS]
        v = 16 * (n // NDMA_SLOTS + 1)
        if v > 16 and self.seen[eng].get(id(s), 0) < v - 16:
            waits.append((s, v - 16))
            self.seen[eng][id(s)] = v - 16
        tok = (s, v, eng, True)
        self.ops[eng].append((waits, emit, s, 16))
        self._commit(tok, reads, writes)
        if is_output:
            self.out_tokens.append(tok)
        return tok

    def barrier(self):
        toks = list(self.latest.values())
        for e in ENG:
            waits = {}
            for (s, v, te, isdma) in toks:
                if self.seen[e].get(id(s), 0) >= v:
                    continue
                if waits.get(id(s), (None, 0))[1] < v:
                    waits[id(s)] = (s, v)
            for (s, v) in waits.values():
                self.seen[e][id(s)] = v
            self.ops[e].append((list(waits.values()), None, None, 0))
        self.last_w = {}
        self.readers = {}

    def finish(self):
        waits = {}
        for (s, v, e, _) in self.out_tokens:
            if waits.get(id(s), (None, 0))[1] < v:
                waits[id(s)] = (s, v)
        self.ops["sp"].append((list(waits.values()), None, None, 0))

    def emit(self):
        nc = self.nc
        ops = self.ops
        waited = {}
        for e in ENG:
            for (waits, emit, s_, inc) in ops[e]:
                for (ws, wv) in waits:
                    if id(ws) in self.eng_sem_ids:
                        waited.setdefault(id(ws), set()).add(wv)
        rank = {sid: {v: r + 1 for r, v in enumerate(sorted(vals))} for sid, vals in waited.items()}
        with nc.Block() as block:
            def replay(engine, lst):
                for (waits, emit, s_, inc) in lst:
                    for (ws, wv) in waits:
                        if id(ws) in self.eng_sem_ids:
                            engine.wait_ge(ws, rank[id(ws)][wv])
                        else:
                            engine.wait_ge(ws, wv)
                    if emit is None:
                        continue
                    ins = emit(engine)
                    if inc < 0:
                        if -inc in rank.get(id(s_), ()):
                            ins.then_inc(s_, 1)
                    else:
                        ins.then_inc(s_, inc)

            @block.sync
            def _(e):
                replay(e, ops["sp"])

            @block.scalar
            def _(e):
                replay(e, ops["act"])

            @block.vector
            def _(e):
                replay(e, ops["dve"])

            @block.gpsimd
            def _(e):
                replay(e, ops["pool"])

            @block.tensor
            def _(e):
                replay(e, ops["pe"])


def _na_tables():
    rl = np.arange(128) // 64
    cc = np.arange(128) % 64
    masks, mask_ids, plan = [], {}, {}
    for b in range(16):
        r = 2 * b + rl
        r0 = np.clip(r - 4, 0, 24)
        c0 = np.clip(cc - 8, 0, 48)
        lst = []
        for j in range(16):
            kr = 2 * j + rl
            ok = ((kr[:, None] >= r0[None, :]) & (kr[:, None] <= r0[None, :] + 7)
                  & (cc[:, None] >= c0[None, :]) & (cc[:, None] <= c0[None, :] + 15))
            if not ok.any():
                continue
            key = ok.tobytes()
            if key not in mask_ids:
                mask_ids[key] = len(masks)
                masks.append(np.where(ok, 0.0, NEG).astype(np.float32))
            assert -3 <= j - b <= 3
            lst.append((j, j - b, mask_ids[key]))
        plan[b] = lst
    return np.stack(masks), plan


def _na_rpb_index():
    rl = np.arange(128) // 64
    cc = np.arange(128) % 64
    dr = np.zeros((7, 128, 128), np.int64)
    dc = np.zeros((7, 128, 128), np.int64)
    for di, delta in enumerate(range(-3, 4)):
        dr[di] = np.clip(2 * delta + rl[:, None] - rl[None, :] + 7, 0, 14)
        dc[di] = np.clip(cc[:, None] - cc[None, :] + 15, 0, 30)
    return dr, dc


def _dil_tables():
    sl = np.arange(128)[:, None]
    tl = np.arange(128)[None, :]
    logm = np.zeros((17, 2, 128, 128), np.float32)
    for di, delta in enumerate(range(-8, 9)):
        o = 128 * delta + sl - tl
        a = np.abs(o)
        m = (a <= 64).astype(np.float64) + ((a <= 256) & (o % 4 == 0)) + ((a <= 1024) & (o % 16 == 0))
        lm = np.where(m > 0, np.log(np.maximum(m, 1)), NEG).astype(np.float32)
        hi = lm.astype(ml_dtypes.bfloat16).astype(np.float32)
        lo = (lm - hi).astype(ml_dtypes.bfloat16).astype(np.float32)
        logm[di, 0] = hi
        logm[di, 1] = lo
    absl = np.stack([(sl - tl), (tl - sl), np.abs(sl - tl)]).astype(np.float32)
    absc = np.stack([np.full((128, 128), 128.0 * k, np.float32) for k in range(9)])
    slopes = (2.0 ** (-8.0 * (np.arange(8) + 1) / 8)).astype(np.float32)
    nsI = np.stack([-slopes[h] * np.eye(128, dtype=np.float32) for h in range(8)])
    return logm, absl, absc, nsI


NA_MASKS, NA_PLAN = _na_tables()
NM = NA_MASKS.shape[0]


def _bf(a):
    return np.ascontiguousarray(a.astype(ml_dtypes.bfloat16))


def build_nc(debug=False):
    nc = bass.Bass("TRN2", target_bir_lowering=False)

    def din(name, shape, dt=F32):
        return nc.dram_tensor(name, list(shape), dt, kind="ExternalInput").ap()

    x_d = din("x", [NB, SEQ, D])
    c_d = din("c", [NB, D])
    adaw_d = din("ada_w", [D, 6 * D])
    adab_d = din("ada_b", [6 * D])
    n1g_d = din("norm1_g", [D])
    n2g_d = din("norm2_g", [D])
    fg_d = din("final_g", [D])
    og_d = din("og", [D])
    win_d = din("w_in", [D, 3 * D])
    wout_d = din("w_out", [D, D])
    wq_d = din("peer_wq", [D, 2 * D])
    sk_d = din("peer_subkeys", [2, 128, 128])
    u_d = din("peer_u", [16384, D])
    v_d = din("peer_v", [16384, D])
    rpbT_d = din("rpbT", [128, 8, 7, 128], BF16)
    nam_d = din("na_masks", [128, NM, 128], BF16)
    logm_d = din("logm", [128, 17, 2, 128], BF16)
    absl_d = din("absl", [128, 3, 128], BF16)
    absc_d = din("absc", [128, 9, 128], BF16)
    nsI_d = din("nsI", [128, 8, 128], BF16)
    idb_d = din("identb", [128, 128], BF16)
    idf_d = din("identf", [128, 128])
    iota_d = din("iota16", [128, 16])
    out_d = nc.dram_tensor("out", [NB, SEQ, D], F32, kind="ExternalOutput").ap()
    x1_d = nc.dram_tensor("x1s", [NB, SEQ, D], F32,
                          kind="ExternalOutput" if debug else "Internal").ap()
    mods_d = nc.dram_tensor("modscr", [NB, 6, D], F32, kind="Internal").ap()
    uv_d = nc.dram_tensor("uvtab", [16384, 2 * D], BF16, kind="Internal").ap()

    with ExitStack() as st:
        S = Sched(nc, st)

        def sb(name, shape, dt):
            return st.enter_context(nc.sbuf_tensor("sb_" + name, list(shape), dt))

        banks = [st.enter_context(nc.psum_tensor(f"bank{i}", [128, 512], F32)) for i in range(8)]
        Sps = [banks[g][:].rearrange("p (c t) -> p c t", c=4) for g in range(3)]
        Ops2 = [banks[3], banks[4]]
        TPb = banks[5][:].bitcast(BF16).rearrange("p (c t) -> p c t", c=8)
        PJ = [banks[6], banks[7]]

        identb = sb("identb", [128, 128], BF16)
        identf = sb("identf", [128, 128], F32)
        iota16 = sb("iota16", [128, 16], F32)
        thr16 = sb("thr16", [128, 16], F32)
        og_bc = sb("og_bc", [128, D], F32)
        fg_bc = og_bc
        modbc = sb("modbc", [128, 3, D], F32)
        xt = [sb(f"xt{i}", [128, D], F32) for i in range(2)]
        junk = sb("junk", [128, D], BF16)
        hb = sb("hb", [128, D], BF16)
        junkb = hb
        t1 = sb("t1", [128, D], F32)
        stat = sb("stat", [128, 8], F32)
        wbuf = sb("wbuf", [128, 8, 2560], BF16)

        BIGW = 34200
        big = sb("big", [128, BIGW], F32)
        off = [0]

        def carve(words, dt=F32, shape=None):
            a = off[0]
            off[0] += words
            assert off[0] <= BIGW, off[0]
            v = big[:, a:a + words]
            if dt != F32:
                v = v.bitcast(dt)
            return v

        def reset_carve():
            off[0] = 0

        S.dma("sp", lambda e: e.dma_start(out=identb[:], in_=idb_d), writes=["identb"])
        S.dma("sp", lambda e: e.dma_start(out=identf[:], in_=idf_d), writes=["identf"])
        S.dma("sp", lambda e: e.dma_start(out=iota16[:], in_=iota_d), writes=["iota16"])
        S.op("dve", lambda e: e.tensor_scalar(out=thr16[:], in0=iota16[:], scalar1=16.0, scalar2=16.0, op0=ALU.mult, op1=ALU.add),
             reads=["iota16"], writes=["thr16"])
        S.dma("sp", lambda e: e.dma_start(out=og_bc[:], in_=og_d.partition_broadcast(128)), writes=["og_bc"])

        reset_carve()
        c2 = carve(D)
        sc2 = carve(D)
        scT = carve(16)
        scT3 = scT.rearrange("p (k b) -> p k b", k=8)
        adab2 = carve(6 * D)
        modsb = carve(6 * D)
        n1g2 = carve(D)
        n2g2 = carve(D)
        awt = [carve(8 * 512).rearrange("p (k n) -> p k n", k=8) for _ in range(2)]

        S.dma("sp", lambda e: e.dma_start(out=c2[0:2, :], in_=c_d), writes=["c2"])
        S.dma("sp", lambda e: e.dma_start(out=adab2[0:2, :], in_=adab_d.partition_broadcast(2)), writes=["adab2"])
        S.dma("sp", lambda e: e.dma_start(out=n1g2[0:2, :], in_=n1g_d.partition_broadcast(2)), writes=["n1g2"])
        S.dma("sp", lambda e: e.dma_start(out=n2g2[0:2, :], in_=n2g_d.partition_broadcast(2)), writes=["n2g2"])
        S.op("act", lambda e: e.activation(out=sc2[0:2, :], in_=c2[0:2, :], func=AF.Silu), reads=["c2"], writes=["sc2"])
        tpf = banks[5][:, 0:16].rearrange("p (k b) -> p k b", k=8)
        for k in range(8):
            S.op("pe", lambda e, k=k: e.transpose(out=tpf[:, k, :], in_=sc2[0:2, k * 128:(k + 1) * 128],
                                                  identity=identf[0:2, 0:2]),
                 reads=["sc2", "identf"], writes=["TP"])
        S.op("dve", lambda e: e.tensor_copy(out=scT3, in_=tpf), reads=["TP"], writes=["scT"])
        adaw_v = adaw_d.rearrange("(k p) n -> p k n", p=128)
        for n in range(12):
            a = awt[n % 2]
            S.dma("sp", lambda e, a=a, n=n: e.dma_start(out=a, in_=adaw_v[:, :, n * 512:(n + 1) * 512]),
                  writes=[("awt", n % 2)])
            pj = PJ[n % 2]
            for k in range(8):
                S.op("pe", lambda e, a=a, k=k, pj=pj: e.matmul(out=pj[0:2, :], lhsT=scT3[:, k, :], rhs=a[:, k, :],
                                                              start=(k == 0), stop=(k == 7)),
                     reads=["scT", ("awt", n % 2)], writes=[("PJ", n % 2)])
            S.op("dve", lambda e, pj=pj, n=n: e.tensor_tensor(out=modsb[0:2, n * 512:(n + 1) * 512], in0=pj[0:2, :],
                                                              in1=adab2[0:2, n * 512:(n + 1) * 512], op=ALU.add),
                 reads=[("PJ", n % 2), "adab2"], writes=["modsb"])
        S.op("dve", lambda e: e.scalar_tensor_tensor(out=modsb[0:2, D:2 * D], in0=modsb[0:2, D:2 * D], scalar=1.0,
                                                     in1=n1g2[0:2, :], op0=ALU.add, op1=ALU.mult),
             reads=["modsb", "n1g2"], writes=["modsb"])
        S.op("dve", lambda e: e.scalar_tensor_tensor(out=modsb[0:2, 4 * D:5 * D], in0=modsb[0:2, 4 * D:5 * D], scalar=1.0,
                                                     in1=n2g2[0:2, :], op0=ALU.add, op1=ALU.mult),
             reads=["modsb", "n2g2"], writes=["modsb"])
        for dst, src in enumerate([1, 0, 2, 4, 3, 5]):
            S.dma("sp", lambda e, dst=dst, src=src: e.dma_start(out=mods_d[:, dst, :], in_=modsb[0:2, src * D:(src + 1) * D]),
                  reads=["modsb"], writes=["modscr"])
        S.barrier()

        def norm_mod(xin, xkey, gm, sh, mkey, out_bf, out_key, out_f32=None, out_f32_key=None):
            S.op("act", lambda e: e.activation(out=junk[:], in_=xin, func=AF.Square, accum_out=stat[:, 0:1]),
                 reads=[xkey], writes=["junk", "stat"])
            S.op("dve", lambda e: e.tensor_scalar(out=stat[:, 1:2], in0=stat[:, 0:1], scalar1=1.0 / D, scalar2=EPS,
                                                  op0=ALU.mult, op1=ALU.add), reads=["stat"], writes=["stat"])
            S.op("act", lambda e: e.sqrt(out=stat[:, 3:4], in_=stat[:, 1:2]), reads=["stat"], writes=["stat"])
            S.op("dve", lambda e: e.reciprocal(out=stat[:, 2:3], in_=stat[:, 3:4]), reads=["stat"], writes=["stat"])
            S.op("dve", lambda e: e.scalar_tensor_tensor(out=t1[:], in0=xin, scalar=stat[:, 2:3], in1=gm,
                                                         op0=ALU.mult, op1=ALU.mult),
                 reads=[xkey, "stat", mkey], writes=["t1"])
            if out_f32 is not None:
                S.op("dve", lambda e: e.tensor_tensor(out=out_f32, in0=t1[:], in1=sh, op=ALU.add),
                     reads=["t1", ("modbc", 1)], writes=[out_f32_key])
                S.op("dve", lambda e: e.tensor_copy(out=out_bf, in_=out_f32), reads=[out_f32_key], writes=[out_key])
            else:
                S.op("dve", lambda e: e.tensor_tensor(out=out_bf, in0=t1[:], in1=sh, op=ALU.add),
                     reads=["t1", ("modbc", 1)], writes=[out_key])

        reset_carve()
        hT = carve(8 * SEQ // 2, BF16).rearrange("p (k t) -> p k t", k=8)
        KT = carve(4 * SEQ // 2, BF16).rearrange("p (k t) -> p k t", k=4)
        Vt = carve(NT * 8 * 66 // 2, BF16).rearrange("p (i h d) -> p i h d", i=NT, h=8)
        QT = carve(2 * 4 * 512 // 2, BF16).rearrange("p (s k t) -> p s k t", s=2, k=4)
        onTa = carve(4 * SEQ // 2, BF16).rearrange("p (k t) -> p k t", k=4)
        onTd = carve(4 * 128 // 2, BF16).rearrange("p (k t) -> p k t", k=4)
        ETs = [carve(4 * 128 // 2, BF16).rearrange("p (c t) -> p c t", c=4) for _ in range(4)]
        otile = carve(512)
        onb = carve(512 // 2, BF16)
        orec = carve(8)
        rpbT = carve(8 * 7 * 128 // 2, BF16).rearrange("p (h d q) -> p h d q", h=8, d=7)
        nam = carve(NM * 128 // 2, BF16).rearrange("p (m q) -> p m q", m=NM)
        logm = carve(17 * 2 * 128 // 2, BF16).rearrange("p (d s q) -> p d s q", d=17, s=2)
        absl = carve(3 * 128 // 2, BF16).rearrange("p (d q) -> p d q", d=3)
        absc = carve(9 * 128 // 2, BF16).rearrange("p (d q) -> p d q", d=9)
        nsI = carve(8 * 128 // 2, BF16).rearrange("p (h q) -> p h q", h=8)

        print("attn carve words", off[0], flush=True)
        cstg = [carve(2 * D // 2, BF16) for _ in range(2)]
        cblk = [0]

        def conv_step(n):
            for _ in range(n):
                blk = cblk[0]
                if blk >= 128:
                    return
                cblk[0] += 1
                r2 = blk % 2
                rows = slice(blk * 128, (blk + 1) * 128)
                S.dma("pool", lambda e, r2=r2, rows=rows: e.dma_start(out=cstg[r2][:, 0:D], in_=u_d[rows, :]), writes=[("cstg", r2, 0)])
                S.dma("pool", lambda e, r2=r2, rows=rows: e.dma_start(out=cstg[r2][:, D:2 * D], in_=v_d[rows, :]), writes=[("cstg", r2, 1)])
                S.dma("pool", lambda e, r2=r2, rows=rows: e.dma_start(out=uv_d[rows, :], in_=cstg[r2]),
                      reads=[("cstg", r2, 0), ("cstg", r2, 1)], writes=["uvtab"])

        S.dma("sp", lambda e: e.dma_start(out=rpbT, in_=rpbT_d), writes=["rpbT"])
        S.dma("sp", lambda e: e.dma_start(out=nam, in_=nam_d), writes=["nam"])
        S.dma("sp", lambda e: e.dma_start(out=logm, in_=logm_d), writes=["logm"])
        S.dma("sp", lambda e: e.dma_start(out=absl, in_=absl_d), writes=["absl"])
        S.dma("sp", lambda e: e.dma_start(out=absc, in_=absc_d), writes=["absc"])
        S.dma("sp", lambda e: e.dma_start(out=nsI, in_=nsI_d), writes=["nsI"])
        S.op("dve", lambda e: e.memset(Vt[:, :, :, 64:66], 1.0), writes=["Vones"])
        S.op("dve", lambda e: e.memset(QT, 0.0), writes=["QTzero"])

        win_v = win_d.rearrange("(k p) n -> p k n", p=128)
        wout_v = wout_d.rearrange("(k p) n -> p k n", p=128)
        wq_v = wq_d.rearrange("(k p) n -> p k n", p=128)

        def load_w(src_v, c0, ncols, dst0, key):
            for k in range(8):
                for cc in range(0, ncols, 512):
                    S.dma("pool", lambda e, k=k, cc=cc: e.dma_start(out=wbuf[:, k, dst0 + cc:dst0 + cc + 512],
                                                                    in_=src_v[:, k, c0 + cc:c0 + cc + 512]),
                          writes=[("W", k, (dst0 + cc) // 512)])

        evac_flip = [0]

        def evac(out_ap, in_ap, reads, writes, scale=None):
            evac_flip[0] ^= 1
            if evac_flip[0]:
                if scale is None:
                    S.op("act", lambda e: e.copy(out=out_ap, in_=in_ap), reads=reads, writes=writes)
                else:
                    S.op("act", lambda e: e.mul(out_ap, in_ap, scale), reads=reads, writes=writes)
            else:
                if scale is None:
                    S.op("dve", lambda e: e.tensor_copy(out=out_ap, in_=in_ap), reads=reads, writes=writes)
                else:
                    S.op("dve", lambda e: e.tensor_scalar(out=out_ap, in0=in_ap, scalar1=scale, scalar2=None,
                                                          op0=ALU.mult), reads=reads, writes=writes)

        pj_i = [0]

        def proj_fm(dst, dkey, wcol0, tok0, ntok, wkey, scale=None):
            p = pj_i[0] % 2
            pj_i[0] += 1
            pj = PJ[p]
            tiles = sorted(set(range(tok0 // 128, (tok0 + ntok) // 128)))
            for k in range(8):
                S.op("pe", lambda e, k=k, pj=pj: e.matmul(out=pj[:, 0:ntok], lhsT=wbuf[:, k, wcol0:wcol0 + 128],
                                                          rhs=hT[:, k, tok0:tok0 + ntok], start=(k == 0), stop=(k == 7)),
                     reads=[("W", k, wcol0 // 512)] + [("hT", i) for i in tiles], writes=[("PJ", p)])
            if isinstance(dst, list):
                for (d_ap, r0, dk) in dst:
                    evac(d_ap, pj[r0:r0 + 64, 0:ntok], [("PJ", p), "QTzero"], [dk], scale)
            else:
                evac(dst, pj[:, 0:ntok], [("PJ", p)], [dkey], scale)

        def proj_v(b_tile, wcol0, wkey):
            p = pj_i[0] % 2
            pj_i[0] += 1
            pj = PJ[p]
            for k in range(8):
                S.op("pe", lambda e, k=k, pj=pj: e.matmul(out=pj[:, :], lhsT=hT[:, k, b_tile * 128:(b_tile + 1) * 128],
                                                          rhs=wbuf[:, k, wcol0:wcol0 + 512], start=(k == 0), stop=(k == 7)),
                     reads=[("W", k, wcol0 // 512), ("hT", b_tile)], writes=[("PJ", p)])
            evac(Vt[:, b_tile, :, 0:64], pj[:, :].rearrange("p (h d) -> p h d", h=8), [("PJ", p)], [("V", b_tile)])

        sring = [0]

        def attention_tile(i, chunks, bias_for, okey, mid=None):
            iq = (i % 4) * 128
            groups = [chunks[a:a + 4] for a in range(0, len(chunks), 4)]
            units = [(h, gi) for h in range(8) for gi in range(len(groups))]
            ring = {}
            LAG = 2

            def scores(h, gi):
                fc = h // 2
                grp = groups[gi]
                bias_mms = bias_for(h)
                g = sring[0] % 3
                sring[0] += 1
                ring[(h, gi)] = g
                for ci, j in enumerate(grp):
                    S.op("pe", lambda e, g=g, ci=ci, j=j, fc=fc, h=h: e.matmul(out=Sps[g][:, ci, :],
                                                                             lhsT=KT[:, fc, j * 128:(j + 1) * 128],
                                                                             rhs=QT[:, h % 2, fc, iq:iq + 128], start=True, stop=False),
                         reads=[("KT", fc, j // 4), ("QT", fc, h % 2), "QTzero"], writes=[("S", g)])
                    bm = bias_mms(j)
                    for bi, (lt, rh, rk) in enumerate(bm):
                        S.op("pe", lambda e, g=g, ci=ci, lt=lt, rh=rh, last=(bi == len(bm) - 1):
                             e.matmul(out=Sps[g][:, ci, :], lhsT=lt, rhs=rh, start=False, stop=last),
                             reads=rk, writes=[("S", g)])
                n = len(grp)
                S.op("act", lambda e, g=g, n=n: e.activation(out=ETs[g][:, 0:n, :], in_=Sps[g][:, 0:n, :], func=AF.Exp),
                     reads=[("S", g)], writes=[("ET", g)])

            def pv(h, gi):
                grp = groups[gi]
                g = ring[(h, gi)]
                slot = h % 2
                for ci, j in enumerate(grp):
                    first = (gi == 0 and ci == 0)
                    last = (gi == len(groups) - 1 and ci == len(grp) - 1)
                    S.op("pe", lambda e, g=g, ci=ci, j=j, h=h, slot=slot, first=first, last=last: e.matmul(
                        out=Ops2[slot][:, 0:66], lhsT=ETs[g][:, ci, :], rhs=Vt[:, j, h, 0:66], start=first, stop=last),
                        reads=[("ET", g), ("V", j), "Vones"], writes=[("O", slot)])
                if gi == len(groups) - 1:
                    S.op("dve", lambda e, h=h, slot=slot: e.reciprocal(out=orec[:, h:h + 1], in_=Ops2[slot][:, 64:65]),
                         reads=[("O", slot)], writes=["orec"])
                    S.op("dve", lambda e, h=h, slot=slot: e.tensor_scalar(out=otile[:, h * 64:(h + 1) * 64], in0=Ops2[slot][:, 0:64],
                                                                          scalar1=orec[:, h:h + 1], scalar2=None, op0=ALU.mult),
                         reads=[("O", slot), "orec"], writes=[okey])

            for k in range(len(units) + LAG):
                if k < len(units):
                    scores(*units[k])
                if k == LAG and mid is not None:
                    mid()
                if k >= LAG:
                    pv(*units[k - LAG])

        def out_norm(okey, gcol0, dstT, dst_cols, dkey):
            S.op("act", lambda e: e.activation(out=junk[:, 0:512], in_=otile, func=AF.Square, accum_out=stat[:, 4:5]),
                 reads=[okey], writes=["junk", "stat2"])
            S.op("dve", lambda e: e.tensor_scalar(out=stat[:, 5:6], in0=stat[:, 4:5], scalar1=1.0 / 512, scalar2=EPS,
                                                  op0=ALU.mult, op1=ALU.add), reads=["stat2"], writes=["stat2"])
            S.op("act", lambda e: e.activation(out=stat[:, 7:8], in_=stat[:, 5:6], func=AF.Ln), reads=["stat2"], writes=["stat2"])
            S.op("act", lambda e: e.activation(out=stat[:, 6:7], in_=stat[:, 7:8], func=AF.Exp, scale=-0.5), reads=["stat2"], writes=["stat2"])
            S.op("dve", lambda e: e.scalar_tensor_tensor(out=onb, in0=otile, scalar=stat[:, 6:7],
                                                         in1=og_bc[:, gcol0:gcol0 + 512], op0=ALU.mult, op1=ALU.mult),
                 reads=[okey, "stat2", "og_bc"], writes=["onb"])
            for k in range(4):
                S.op("pe", lambda e, k=k: e.transpose(out=TPb[:, k, :], in_=onb[:, k * 128:(k + 1) * 128], identity=identb[:]),
                     reads=["onb", "identb"], writes=["TP"])
            evac(dstT[:, :, dst_cols], TPb[:, 0:4, :], ["TP"], [dkey])

        def na_bias(i, h):
            plan = {j: (dl, mid) for (j, dl, mid) in NA_PLAN[i]}

            def f(j):
                dl, mid = plan[j]
                return [(identb[:], rpbT[:, h, dl + 3, :], ["identb", "rpbT"]),
                        (identb[:], nam[:, mid, :], ["identb", "nam"])]
            return f

        def dil_bias(i, h):
            def f(j):
                dl = j - i
                sg = 0 if dl > 0 else (1 if dl < 0 else 2)
                mm = [(nsI[:, h, :], absl[:, sg, :], ["nsI", "absl"])]
                if dl != 0:
                    mm.append((nsI[:, h, :], absc[:, abs(dl), :], ["nsI", "absc"]))
                mm.append((identb[:], logm[:, dl + 8, 0, :], ["identb", "logm"]))
                if abs(dl) <= 2:
                    mm.append((identb[:], logm[:, dl + 8, 1, :], ["identb", "logm"]))
                return mm
            return f

        xcount = [0]
        epi = [None]
        for b in range(NB):
            for vi in range(3):
                S.dma("sp", lambda e, b=b, vi=vi: e.dma_start(out=modbc[:, vi, :], in_=mods_d[b, vi, :].partition_broadcast(128)),
                      reads=["modscr"], writes=[("modbc", vi)])
            for i in range(NT):
                xb = xt[xcount[0] % 2]
                xk = ("xt", xcount[0] % 2)
                xcount[0] += 1
                S.dma("sp", lambda e, b=b, xb=xb, i=i: e.dma_start(out=xb[:], in_=x_d[b, i * 128:(i + 1) * 128, :]), writes=[xk])
                norm_mod(xb[:], xk, modbc[:, 0, :], modbc[:, 1, :], ("modbc", 0), hb[:], "hb")
                for k in range(8):
                    S.op("pe", lambda e, k=k: e.transpose(out=TPb[:, k, :], in_=hb[:, k * 128:(k + 1) * 128], identity=identb[:]),
                         reads=["hb", "identb"], writes=["TP"])
                evac(hT[:, :, i * 128:(i + 1) * 128], TPb[:, :, :], ["TP"], [("hT", i)])
            load_w(win_v, 0, 1536, 0, "wA")
            for fc in range(4):
                for g4 in range(4):
                    proj_fm(KT[:, fc, g4 * 512:(g4 + 1) * 512], ("KT", fc, g4), 512 + fc * 128, g4 * 512, 512, "wA")
            for i in range(NT):
                proj_v(i, 1024, "wA")
            for i in range(NT):
                if i % 4 == 0:
                    for fc in range(4):
                        proj_fm([(QT[0:64, 0, fc, :], 0, ("QT", fc, 0)), (QT[64:128, 1, fc, :], 64, ("QT", fc, 1))], None, fc * 128, i * 128, 512, "wA", scale=0.125)
                conv_step(2)
                attention_tile(i, [j for (j, _, _) in NA_PLAN[i]], lambda h, i=i: na_bias(i, h), "otile", mid=epi[0])
                epi[0] = (lambda i=i: out_norm("otile", 0, onTa, slice(i * 128, (i + 1) * 128), ("onTa", i)))
            epi[0]()
            epi[0] = None
            load_w(win_v, 1536, 1536, 0, "wA")
            load_w(wout_v, 0, 1024, 1536, "wO")
            for fc in range(4):
                for g4 in range(4):
                    proj_fm(KT[:, fc, g4 * 512:(g4 + 1) * 512], ("KT", fc, g4), 512 + fc * 128, g4 * 512, 512, "wA")
            for i in range(NT):
                proj_v(i, 1024, "wA")
            for i in range(NT):
                if i % 4 == 0:
                    for fc in range(4):
                        proj_fm([(QT[0:64, 0, fc, :], 0, ("QT", fc, 0)), (QT[64:128, 1, fc, :], 64, ("QT", fc, 1))], None, fc * 128, i * 128, 512, "wA", scale=0.125)
                conv_step(2)
                chunks = [j for j in range(NT) if abs(j - i) <= 8]
                attention_tile(i, chunks, lambda h, i=i: dil_bias(i, h), "otile", mid=epi[0])

                def _epi(b=b, i=i):
                    out_norm("otile", 512, onTd, slice(0, 128), "onTd")
                    for half in range(2):
                        pj = PJ[half]
                        for k in range(8):
                            lt = onTa[:, k, i * 128:(i + 1) * 128] if k < 4 else onTd[:, k - 4, :]
                            rk = [("onTa", i)] if k < 4 else ["onTd"]
                            S.op("pe", lambda e, k=k, pj=pj, lt=lt, half=half: e.matmul(
                                out=pj[:, :], lhsT=lt, rhs=wbuf[:, k, 1536 + half * 512:1536 + (half + 1) * 512],
                                start=(k == 0), stop=(k == 7)), reads=rk + [("W", k, 3 + half)], writes=[("PJ", half)])
                    xb = xt[xcount[0] % 2]
                    xk = ("xt", xcount[0] % 2)
                    xcount[0] += 1
                    S.dma("sp", lambda e, b=b, xb=xb, i=i: e.dma_start(out=xb[:], in_=x_d[b, i * 128:(i + 1) * 128, :]), writes=[xk])
                    for half in range(2):
                        S.op("dve", lambda e, half=half: e.tensor_tensor(out=t1[:, half * 512:(half + 1) * 512], in0=PJ[half][:, :],
                                                                          in1=modbc[:, 2, half * 512:(half + 1) * 512], op=ALU.mult),
                             reads=[("PJ", half), ("modbc", 2)], writes=["t1"])
                    S.op("dve", lambda e, xb=xb: e.tensor_tensor(out=xb[:], in0=xb[:], in1=t1[:], op=ALU.add),
                         reads=[xk, "t1"], writes=[xk])
                    S.dma("sp", lambda e, b=b, xb=xb, i=i: e.dma_start(out=x1_d[b, i * 128:(i + 1) * 128, :], in_=xb[:]),
                          reads=[xk], writes=[("x1s", b, i)], is_output=debug)

                epi[0] = _epi
            epi[0]()
            epi[0] = None
        conv_step(128)
        load_w(wq_v, 0, 2048, 0, "wQ")
        S.barrier()

        reset_carve()
        h2b = [carve(D // 2, BF16) for _ in range(2)]
        h2T = carve(8 * 128 // 2, BF16).rearrange("p (k t) -> p k t", k=8)
        qTs = carve(16 * 128).rearrange("p (c t) -> p c t", c=16)
        skT = carve(2 * 128).rearrange("p (s n) -> p s n", s=2)
        sks = carve(2 * 128).rearrange("p (s n) -> p s n", s=2)
        scs = carve(2048).rearrange("p (g n) -> p g n", g=16)
        sc2_ = carve(128)
        tv = carve(256).rearrange("p (g k) -> p g k", g=16)
        ti = carve(256, U32).rearrange("p (g k) -> p g k", g=16)
        tif = carve(256).rearrange("p (h s k) -> p h s k", h=8, s=2)
        cand = carve(2048).rearrange("p (h a b) -> p h a b", h=8, a=16)
        cand2 = carve(256)
        bv = carve(128).rearrange("p (h k) -> p h k", h=8)
        bp = carve(128, U32).rearrange("p (h k) -> p h k", h=8)
        kif = carve(128).rearrange("p (h k) -> p h k", h=8)
        kjf = carve(128).rearrange("p (h k) -> p h k", h=8)
        eq = cand
        If = carve(128).rearrange("p (h k) -> p h k", h=8)
        Jf = carve(128).rearrange("p (h k) -> p h k", h=8)
        idsf = carve(128)
        ids2 = [carve(128, U32) for _ in range(2)]
        gsum = carve(8)
        gates2 = [carve(128).rearrange("p (h k) -> p h k", h=8) for _ in range(2)]
        adot = carve(128)
        gl = carve(128)
        wg = carve(128)
        NSLOT = 17
        uvg = [carve(2 * D // 2, BF16) for _ in range(NSLOT)]
        prod = [carve(D // 2, BF16) for _ in range(4)]
        dg = [carve(64, BF16) for _ in range(8)]
        modbcP = [modbc, carve(3 * D).rearrange("p (v d) -> p v d", v=3)]

        QP = banks[0][:].rearrange("p (c t) -> p c t", c=4)
        SCP = [banks[1 + q][:].rearrange("p (c t) -> p c t", c=4) for q in range(4)]
        YP = [banks[6], banks[7]]

        S.dma("sp", lambda e: e.dma_start(out=fg_bc[:], in_=fg_d.partition_broadcast(128)), writes=["fg_bc"])
        S.dma("sp", lambda e: e.dma_start(out=sks, in_=sk_d.rearrange("s n d -> n s d")), writes=["sks"])
        tpf2 = banks[5][:, 0:256].rearrange("p (s n) -> p s n", s=2)
        for s_ in range(2):
            S.op("pe", lambda e, s_=s_: e.transpose(out=tpf2[:, s_, :], in_=sks[:, s_, :], identity=identf[:]),
                 reads=["sks", "identf"], writes=["TP"])
        S.op("dve", lambda e: e.tensor_copy(out=skT, in_=tpf2), reads=["TP"], writes=["skT"])
        for b in range(NB):
            for vi in range(3):
                S.dma("sp", lambda e, b=b, vi=vi: e.dma_start(out=modbcP[b][:, vi, :], in_=mods_d[b, 3 + vi, :].partition_broadcast(128)),
                      writes=[("modbcP", b, vi)])

        def peer_front(b, i, par):
            xb = xt[par]
            xk = ("xt", par)
            ids = ids2[par]
            gates = gates2[par]
            hbk = ("h2b", par)
            mb = modbcP[b]
            S.dma("sp", lambda e: e.dma_start(out=xb[:], in_=x1_d[b, i * 128:(i + 1) * 128, :]), writes=[xk])
            S.op("act", lambda e: e.activation(out=junk[:], in_=xb[:], func=AF.Square, accum_out=stat[:, 0:1]),
                 reads=[xk], writes=["junk", "stat"])
            S.op("dve", lambda e: e.tensor_scalar(out=stat[:, 1:2], in0=stat[:, 0:1], scalar1=1.0 / D, scalar2=EPS,
                                                  op0=ALU.mult, op1=ALU.add), reads=["stat"], writes=["stat"])
            S.op("act", lambda e: e.sqrt(out=stat[:, 3:4], in_=stat[:, 1:2]), reads=["stat"], writes=["stat"])
            S.op("dve", lambda e: e.reciprocal(out=stat[:, 2:3], in_=stat[:, 3:4]), reads=["stat"], writes=["stat"])
            S.op("dve", lambda e: e.scalar_tensor_tensor(out=t1[:], in0=xb[:], scalar=stat[:, 2:3], in1=mb[:, 0, :],
                                                         op0=ALU.mult, op1=ALU.mult),
                 reads=[xk, "stat", ("modbcP", b, 0)], writes=["t1"])
            S.op("dve", lambda e: e.tensor_tensor(out=h2b[par], in0=t1[:], in1=mb[:, 1, :], op=ALU.add),
                 reads=["t1", ("modbcP", b, 1)], writes=[hbk])
            for k in range(8):
                S.op("pe", lambda e, k=k: e.transpose(out=TPb[:, k, :], in_=h2b[par][:, k * 128:(k + 1) * 128], identity=identb[:]),
                     reads=[hbk, "identb"], writes=["TP"])
            evac(h2T, TPb[:, :, :], ["TP"], ["h2T"])
            for c4 in range(4):
                for cc in range(4):
                    c = c4 * 4 + cc
                    for k in range(8):
                        S.op("pe", lambda e, c=c, cc=cc, k=k: e.matmul(out=QP[:, cc, :], lhsT=wbuf[:, k, c * 128:(c + 1) * 128],
                                                                       rhs=h2T[:, k, :], start=(k == 0), stop=(k == 7)),
                             reads=[("W", k, c // 4), "h2T"], writes=["QP"])
                evac(qTs[:, c4 * 4:(c4 + 1) * 4, :], QP, ["QP"], [("qTs", c4)])
            for q4 in range(4):
                for cc in range(4):
                    c = q4 * 4 + cc
                    S.op("pe", lambda e, c=c, cc=cc, q4=q4: e.matmul(out=SCP[q4][:, cc, :], lhsT=qTs[:, c, :], rhs=skT[:, c % 2, :],
                                                                     start=True, stop=True),
                         reads=[("qTs", q4), "skT"], writes=[("SCP", q4)])
                S.op("act", lambda e, q4=q4: e.copy(out=scs[:, q4 * 4:(q4 + 1) * 4, :], in_=SCP[q4]),
                     reads=[("SCP", q4)], writes=[("scs", q4)])
            for g in range(16):
                rk = [("scs", g // 4)]
                S.op("dve", lambda e, g=g: e.max(out=tv[:, g, 0:8], in_=scs[:, g, :]), reads=rk, writes=["tv"])
                S.op("dve", lambda e, g=g: e.max_index(out=ti[:, g, 0:8], in_max=tv[:, g, 0:8], in_values=scs[:, g, :]),
                     reads=rk + ["tv"], writes=["ti"])
                S.op("dve", lambda e, g=g: e.match_replace(out=sc2_, in_to_replace=tv[:, g, 0:8], in_values=scs[:, g, :],
                                                           imm_value=-1e30), reads=rk + ["tv"], writes=["sc2_"])
                S.op("dve", lambda e, g=g: e.max(out=tv[:, g, 8:16], in_=sc2_), reads=["sc2_"], writes=["tv"])
                S.op("dve", lambda e, g=g: e.max_index(out=ti[:, g, 8:16], in_max=tv[:, g, 8:16], in_values=sc2_),
                     reads=["sc2_", "tv"], writes=["ti"])
            S.op("dve", lambda e: e.tensor_copy(out=tif.rearrange("p h s k -> p (h s k)"),
                                                in_=ti.rearrange("p g k -> p (g k)")), reads=["ti"], writes=["tif"])
            tvv = tv.rearrange("p (h s) k -> p h s k", s=2)
            S.op("dve", lambda e: e.tensor_tensor(out=cand, in0=tvv[:, :, 0, :].unsqueeze(3).to_broadcast([128, 8, 16, 16]),
                                                  in1=tvv[:, :, 1, :].unsqueeze(2).to_broadcast([128, 8, 16, 16]), op=ALU.add),
                 reads=["tv"], writes=["cand"], cost=2.3)
            for h in range(8):
                ch = cand[:, h, :, :].rearrange("p a b -> p (a b)")
                S.op("dve", lambda e, h=h, ch=ch: e.max(out=bv[:, h, 0:8], in_=ch), reads=["cand"], writes=["bv"])
                S.op("dve", lambda e, h=h, ch=ch: e.max_index(out=bp[:, h, 0:8], in_max=bv[:, h, 0:8], in_values=ch),
                     reads=["cand", "bv"], writes=["bp"])
                S.op("dve", lambda e, h=h, ch=ch: e.match_replace(out=cand2, in_to_replace=bv[:, h, 0:8], in_values=ch,
                                                                  imm_value=-1e30), reads=["cand", "bv"], writes=["cand2"])
                S.op("dve", lambda e, h=h: e.max(out=bv[:, h, 8:16], in_=cand2), reads=["cand2"], writes=["bv"])
                S.op("dve", lambda e, h=h: e.max_index(out=bp[:, h, 8:16], in_max=bv[:, h, 8:16], in_values=cand2),
                     reads=["cand2", "bv"], writes=["bp"])
            S.op("dve", lambda e: e.tensor_copy(out=If, in_=bp), reads=["bp"], writes=["If"])
            thb = thr16[:].unsqueeze(1).unsqueeze(1).to_broadcast([128, 8, 16, 16])
            S.op("dve", lambda e: e.tensor_tensor(out=eq, in0=If.unsqueeze(3).to_broadcast([128, 8, 16, 16]), in1=thb, op=ALU.is_ge),
                 reads=["If", "thr16"], writes=["eq"], cost=2.3)
            S.op("dve", lambda e: e.tensor_reduce(out=kif, in_=eq, axis=AX.X, op=ALU.add), reads=["eq"], writes=["kif"], cost=2.3)
            S.op("dve", lambda e: e.scalar_tensor_tensor(out=kjf, in0=kif, scalar=-16.0, in1=If, op0=ALU.mult, op1=ALU.add),
                 reads=["kif", "If"], writes=["kjf"])
            iob = iota16[:].unsqueeze(1).unsqueeze(1).to_broadcast([128, 8, 16, 16])
            for (kf, kkey, sidx, dst, dkey) in ((kif, "kif", 0, If, "If"), (kjf, "kjf", 1, Jf, "Jf")):
                S.op("dve", lambda e, kf=kf: e.tensor_tensor(out=eq, in0=kf.unsqueeze(3).to_broadcast([128, 8, 16, 16]),
                                                             in1=iob, op=ALU.is_equal), reads=[kkey, "iota16"], writes=["eq"], cost=2.3)
                S.op("dve", lambda e, sidx=sidx: e.tensor_tensor(out=eq, in0=eq,
                                                                 in1=tif[:, :, sidx, :].unsqueeze(2).to_broadcast([128, 8, 16, 16]),
                                                                 op=ALU.mult), reads=["eq", "tif"], writes=["eq"], cost=2.3)
                S.op("dve", lambda e, dst=dst: e.tensor_reduce(out=dst, in_=eq, axis=AX.X, op=ALU.add),
                     reads=["eq", "kjf"], writes=[dkey], cost=2.3)
            S.op("dve", lambda e: e.scalar_tensor_tensor(out=idsf, in0=If.rearrange("p h k -> p (h k)"), scalar=128.0,
                                                         in1=Jf.rearrange("p h k -> p (h k)"), op0=ALU.mult, op1=ALU.add),
                 reads=["If", "Jf"], writes=["idsf"])
            S.op("dve", lambda e: e.tensor_scalar(out=idsf, in0=idsf, scalar1=16383.0, scalar2=0.0, op0=ALU.min, op1=ALU.max),
                 reads=["idsf"], writes=["idsf"])
            S.op("dve", lambda e: e.tensor_copy(out=ids, in_=idsf), reads=["idsf"], writes=[("ids", par)])
            gk = ("gates", par)
            S.op("dve", lambda e: e.tensor_tensor(out=gates, in0=bv, in1=bv[:, :, 0:1].to_broadcast([128, 8, 16]), op=ALU.subtract),
                 reads=["bv"], writes=[gk])
            S.op("act", lambda e: e.activation(out=gates, in_=gates, func=AF.Exp), reads=[gk], writes=[gk])
            S.op("dve", lambda e: e.tensor_reduce(out=gsum, in_=gates, axis=AX.X, op=ALU.add), reads=[gk], writes=["gsum"])
            S.op("dve", lambda e: e.reciprocal(out=gsum, in_=gsum), reads=["gsum"], writes=["gsum"])
            S.op("dve", lambda e: e.tensor_tensor(out=gates, in0=gates, in1=gsum.unsqueeze(2).to_broadcast([128, 8, 16]), op=ALU.mult),
                 reads=[gk, "gsum"], writes=[gk])

        tiles = [(b, i) for b in range(NB) for i in range(NT)]
        S.defer = []
        peer_front(tiles[0][0], tiles[0][1], 0)
        pending = S.defer
        S.defer = None
        S.run_deferred(pending, len(pending))
        gcount = [0]
        pcount = [0]
        print('peer carve words', off[0], flush=True)
        for ti_, (b, i) in enumerate(tiles):
            par = ti_ % 2
            nxt = []
            if ti_ + 1 < len(tiles):
                S.defer = []
                peer_front(tiles[ti_ + 1][0], tiles[ti_ + 1][1], 1 - par)
                nxt = S.defer
                S.defer = None
            per_slot = (len(nxt) + 111) // 112
            budget = sum(t[3] for t in nxt) / 27.0 if nxt else 0.0
            ids = ids2[par]
            gates = gates2[par].rearrange("p h k -> p (h k)")
            GB = 4

            def gathers(e0):
                sls = []
                for e_ in range(e0, e0 + GB):
                    sl = gcount[0] % NSLOT
                    gcount[0] += 1
                    sls.append(sl)
                    S.dma("pool", lambda e, e_=e_, sl=sl, ids=ids: e.indirect_dma_start(
                        out=uvg[sl], out_offset=None, in_=uv_d, in_offset=bass.IndirectOffsetOnAxis(ap=ids[:, e_:e_ + 1], axis=0)),
                        reads=[("ids", par)], writes=[("uvg", sl)])
                return sls

            def dots(e0, sls):
                for n_, e_ in enumerate(range(e0, e0 + GB)):
                    sl = sls[n_]
                    ak = ("adot", (e_ // GB) % 2, e_ % 4)
                    pr = pcount[0] % 4
                    pcount[0] += 1
                    S.op("dve", lambda e, sl=sl, par=par, pr=pr: e.tensor_tensor(out=prod[pr], in0=uvg[sl][:, 0:D], in1=h2b[par], op=ALU.mult),
                         reads=[("uvg", sl), ("h2b", par)], writes=[("prod", pr)])
                    S.op("act", lambda e, e_=e_, pr=pr: e.activation(out=junk[:], in_=prod[pr], func=AF.Copy, accum_out=adot[:, e_:e_ + 1]),
                         reads=[("prod", pr)], writes=["junk", ak])

            def gelu_stage(e0, sls):
                gb = (e0 // GB) % 2
                S.op("act", lambda e, e0=e0: e.activation(out=gl[:, e0:e0 + GB], in_=adot[:, e0:e0 + GB], func=AF.Gelu),
                     reads=[("adot", gb, 0), ("adot", gb, 1), ("adot", gb, 2), ("adot", gb, 3)], writes=[("gl", gb)])

            def combine(e0, sls):
                gb = (e0 // GB) % 2
                S.op("dve", lambda e, e0=e0, gates=gates: e.tensor_tensor(out=wg[:, e0 + 2:e0 + 4], in0=gl[:, e0 + 2:e0 + 4],
                                                                           in1=gates[:, e0 + 2:e0 + 4], op=ALU.mult),
                     reads=[("gl", gb), ("gates", par)], writes=[("wg", gb)])
                for n_, e_ in enumerate(range(e0, e0 + GB)):
                    sl = sls[n_]
                    d_ = e_ % 8
                    if n_ < 2:
                        S.op("dve", lambda e, e_=e_, d_=d_, gates=gates: e.tensor_scalar(out=dg[d_], in0=identb[:], scalar1=gl[:, e_:e_ + 1],
                                                                                         scalar2=gates[:, e_:e_ + 1], op0=ALU.mult, op1=ALU.mult),
                             reads=[("gl", gb), ("gates", par), "identb"], writes=[("dg", d_)])
                    else:
                        S.op("act", lambda e, e_=e_, d_=d_: e.activation(out=dg[d_], in_=identb[:], func=AF.Copy, scale=wg[:, e_:e_ + 1]),
                             reads=[("wg", gb), "identb"], writes=[("dg", d_)])
                    for half in range(2):
                        S.op("pe", lambda e, e_=e_, sl=sl, d_=d_, half=half: e.matmul(
                            out=YP[half][:, :], lhsT=dg[d_], rhs=uvg[sl][:, D + half * 512:D + (half + 1) * 512],
                            start=(e_ == 0), stop=(e_ == 127)), reads=[("dg", d_), ("uvg", sl)], writes=[("YP", half)])

            prev = None
            for e0 in range(0, 128, GB):
                sls = gathers(e0)
                if prev is not None:
                    gelu_stage(*prev)
                dots(e0, sls)
                if prev is not None:
                    combine(*prev)
                prev = (e0, sls)
                if e0 >= 8:
                    S.run_deferred_budget(nxt, budget)
            gelu_stage(*prev)
            combine(*prev)
            S.run_deferred(nxt, len(nxt))
            xb = xt[par]
            xk = ("xt", par)
            mb = modbcP[b]
            for half in range(2):
                S.op("dve", lambda e, half=half, mb=mb: e.tensor_tensor(out=t1[:, half * 512:(half + 1) * 512], in0=YP[half][:, :],
                                                                         in1=mb[:, 2, half * 512:(half + 1) * 512], op=ALU.mult),
                     reads=[("YP", half), ("modbcP", b, 2)], writes=["t1"])
            S.op("dve", lambda e, xb=xb: e.tensor_tensor(out=xb[:], in0=xb[:], in1=t1[:], op=ALU.add),
                 reads=[xk, "t1"], writes=[xk])
            S.op("act", lambda e, xb=xb: e.activation(out=junk[:], in_=xb[:], func=AF.Square, accum_out=stat[:, 4:5]),
                 reads=[xk], writes=["junk", "stat2"])
            S.op("dve", lambda e: e.tensor_scalar(out=stat[:, 5:6], in0=stat[:, 4:5], scalar1=1.0 / D, scalar2=EPS,
                                                  op0=ALU.mult, op1=ALU.add), reads=["stat2"], writes=["stat2"])
            S.op("act", lambda e: e.sqrt(out=stat[:, 7:8], in_=stat[:, 5:6]), reads=["stat2"], writes=["stat2"])
            S.op("dve", lambda e: e.reciprocal(out=stat[:, 6:7], in_=stat[:, 7:8]), reads=["stat2"], writes=["stat2"])
            S.op("dve", lambda e, xb=xb: e.scalar_tensor_tensor(out=xb[:], in0=xb[:], scalar=stat[:, 6:7], in1=fg_bc[:],
                                                                op0=ALU.mult, op1=ALU.mult),
                 reads=[xk, "stat2", "fg_bc"], writes=[xk])
            S.dma("sp", lambda e, b=b, i=i, xb=xb: e.dma_start(out=out_d[b, i * 128:(i + 1) * 128, :], in_=xb[:]),
                  reads=[xk], writes=[("out", b, i)], is_output=True)
        S.finish()
        S.emit()
        print("ops per engine:", {k: len(v) for k, v in S.ops.items()}, "sems:", S.nsem, flush=True)
    return nc


_CONSTS = None


def _consts(na_rpb):
    global _CONSTS
    if _CONSTS is None:
        logm, absl, absc, nsI = _dil_tables()
        dr, dc = _na_rpb_index()
        _CONSTS = dict(
            na_masks=_bf(NA_MASKS.transpose(1, 0, 2)),
            logm=_bf(logm.transpose(2, 0, 1, 3)),
            absl=_bf(absl.transpose(1, 0, 2)),
            absc=_bf(absc.transpose(1, 0, 2)),
            nsI=_bf(nsI.transpose(1, 0, 2)),
            identb=_bf(np.eye(128, dtype=np.float32)),
            identf=np.eye(128, dtype=np.float32),
            iota16=np.ascontiguousarray(np.broadcast_to(np.arange(16, dtype=np.float32), (128, 16))),
            _dr=dr, _dc=dc,
        )
    cst = {k: v for k, v in _CONSTS.items() if not k.startswith("_")}
    rp = np.asarray(na_rpb, np.float32)[0]
    g = rp[:, _CONSTS["_dr"], _CONSTS["_dc"]]
    cst["rpbT"] = _bf(g.transpose(2, 0, 1, 3))
    return cst


def kernel(x, c, ada_w, ada_b, norm1_g, w_in, na_rpb, out_norm_na_g, out_norm_dil_g, w_out, norm2_g,
           peer_wq, peer_subkeys, peer_u, peer_v, final_g, _debug=False, _cores=None):
    f = lambda a: np.ascontiguousarray(np.asarray(a, dtype=np.float32))
    x = f(x); c = f(c)
    shared = dict(
        ada_w=f(ada_w)[0], ada_b=f(ada_b)[0], norm1_g=f(norm1_g)[0], norm2_g=f(norm2_g)[0], final_g=f(final_g),
        og=np.ascontiguousarray(np.concatenate([f(out_norm_na_g)[0], f(out_norm_dil_g)[0]])),
        w_in=f(w_in)[0], w_out=f(w_out)[0], peer_wq=f(peer_wq)[0], peer_subkeys=f(peer_subkeys)[0],
        peer_u=f(peer_u)[0], peer_v=f(peer_v)[0],
    )
    shared.update(_consts(na_rpb))
    cores = list(range(NCORES)) if _cores is None else _cores
    nc = build_nc(debug=_debug)
    in_maps = []
    for ci in cores:
        m = dict(shared)
        m["x"] = np.ascontiguousarray(x[ci * NB:(ci + 1) * NB])
        m["c"] = np.ascontiguousarray(c[ci * NB:(ci + 1) * NB])
        in_maps.append(m)
    res = run_bass_kernel_spmd(nc, in_maps, core_ids=list(range(len(cores))))
    out = np.concatenate([np.asarray(r["out"]) for r in res.results], axis=0).astype(np.float32)
    if _debug:
        return out, [r for r in res.results]
    return out
```
